# Optimizing a Trainium2 kernel written in Bass

```python
import jax, jax.numpy as jnp
from jax import lax
import numpy as np

D_MODEL = 2048
BATCH = 2
SEQ = 16384
DEPTH = 2

CONV_WIDTH = D_MODEL // 2
CONV_GROUPS = 8
CONV_K = 3
SGU_WIDTH = D_MODEL // 2
SGU_GROUPS = 8
SGU_GROUP_DIM = SGU_WIDTH // SGU_GROUPS
SGU_CHUNK = 128
MIX_IN_WIDTH = 3 * CONV_WIDTH + 2 * SGU_WIDTH
N_HEADS = 16
HEAD_DIM = D_MODEL // N_HEADS
MOBA_BLOCK = 256
MOBA_TOPK = 3
Q_CHUNK = 32
D_FF = 4 * D_MODEL
N_EVEN = (DEPTH + 1) // 2
N_ODD = DEPTH // 2
EPS = 1e-6

kernel_name = "hybrid_conv_sgu_moba_block"


def rms_norm(x, g):
    xf = x.astype(jnp.float32)
    y = xf * lax.rsqrt(jnp.mean(xf * xf, axis=-1, keepdims=True) + EPS)
    return (y * g.astype(jnp.float32)).astype(x.dtype)


def alibi_slopes():
    return 2.0 ** (-8.0 * jnp.arange(1, N_HEADS + 1, dtype=jnp.float32) / N_HEADS)


def conv_sgu_mixer(h, w_in, conv_w, sgu_gain, w_s, b_s, w_out):
    B_, S, _ = h.shape
    z = h @ w_in
    gate_b, gate_c, xa, u, v = jnp.split(
        z, [CONV_WIDTH, 2 * CONV_WIDTH, 3 * CONV_WIDTH, 3 * CONV_WIDTH + SGU_WIDTH], axis=-1)
    y = gate_c * xa
    yp = jnp.pad(y, ((0, 0), (CONV_K - 1, 0), (0, 0)))
    conv = yp[:, 0:S, :] * conv_w[:, 0]
    for tap in range(1, CONV_K):
        conv = conv + yp[:, tap:tap + S, :] * conv_w[:, tap]
    a_out = gate_b * conv
    u = jax.nn.gelu(u)
    v = jax.nn.gelu(v).reshape(B_, S // SGU_CHUNK, SGU_CHUNK, SGU_GROUPS, SGU_GROUP_DIM)
    v = rms_norm(v, sgu_gain.reshape(SGU_GROUPS, SGU_GROUP_DIM))
    causal = jnp.tril(jnp.ones((SGU_CHUNK, SGU_CHUNK), dtype=bool))
    w_causal = jnp.where(causal[None], w_s, jnp.zeros_like(w_s))
    mixed = jnp.einsum('gij,bcjgd->bcigd', w_causal, v) + b_s.T[None, None, :, :, None]
    b_out = u * mixed.reshape(B_, S, SGU_WIDTH)
    return jnp.concatenate([a_out, b_out], axis=-1) @ w_out


def moba_attention(q, k, v):
    B_, H, S, hd = q.shape
    s_pad = -(-S // MOBA_BLOCK) * MOBA_BLOCK
    pad = ((0, 0), (0, 0), (0, s_pad - S), (0, 0))
    q, k, v = jnp.pad(q, pad), jnp.pad(k, pad), jnp.pad(v, pad)
    nb = s_pad // MOBA_BLOCK
    topk = min(MOBA_TOPK, nb)
    k_blocks = k.reshape(B_, H, nb, MOBA_BLOCK, hd)
    v_blocks = v.reshape(B_, H, nb, MOBA_BLOCK, hd)
    k_mean = jnp.mean(k_blocks.astype(jnp.float32), axis=3)
    scale = HEAD_DIM ** -0.5
    slopes = alibi_slopes()
    b_ix = jnp.arange(B_)[:, None, None, None]
    h_ix = jnp.arange(H)[None, :, None, None]
    blk_off = jnp.arange(MOBA_BLOCK)

    def chunk(c):
        q0 = c * Q_CHUNK
        qc = lax.dynamic_slice_in_dim(q, q0, Q_CHUNK, axis=2)
        t = q0 + jnp.arange(Q_CHUNK)
        own = q0 // MOBA_BLOCK
        gate = jnp.einsum('bhqd,bhnd->bhqn', qc.astype(jnp.float32), k_mean)
        gate = jnp.where(jnp.arange(nb) < own, gate, -jnp.inf)
        _, idx = lax.top_k(gate, topk)
        valid = jnp.arange(topk) < own
        kg = k_blocks[b_ix, h_ix, idx]
        vg = v_blocks[b_ix, h_ix, idx]
        kpos = idx[..., None] * MOBA_BLOCK + blk_off
        dist_sel = (t[None, None, :, None, None] - kpos).astype(jnp.float32)
        s_sel = (jnp.einsum('bhqd,bhqjkd->bhqjk', qc, kg).astype(jnp.float32) * scale
                 - slopes[None, :, None, None, None] * dist_sel)
        s_sel = jnp.where(valid[:, None], s_sel, -jnp.inf).reshape(B_, H, Q_CHUNK, topk * MOBA_BLOCK)
        k_own = lax.dynamic_index_in_dim(k_blocks, own, axis=2, keepdims=False)
        v_own = lax.dynamic_index_in_dim(v_blocks, own, axis=2, keepdims=False)
        own_pos = own * MOBA_BLOCK + blk_off
        dist_own = (t[:, None] - own_pos[None, :]).astype(jnp.float32)
        s_own = (jnp.einsum('bhqd,bhkd->bhqk', qc, k_own).astype(jnp.float32) * scale
                 - slopes[None, :, None, None] * dist_own)
        s_own = jnp.where(own_pos[None, :] <= t[:, None], s_own, -jnp.inf)
        p = jax.nn.softmax(jnp.concatenate([s_sel, s_own], axis=-1), axis=-1)
        p_sel = p[..., :topk * MOBA_BLOCK].reshape(B_, H, Q_CHUNK, topk, MOBA_BLOCK).astype(v.dtype)
        p_own = p[..., topk * MOBA_BLOCK:].astype(v.dtype)
        return (jnp.einsum('bhqjk,bhqjkd->bhqd', p_sel, vg)
                + jnp.einsum('bhqk,bhkd->bhqd', p_own, v_own))

    outs = lax.map(chunk, jnp.arange(s_pad // Q_CHUNK))
    out = outs.transpose(1, 0, 3, 2, 4).reshape(B_, s_pad, H * hd)
    return out[:, :S]


def moba_mixer(h, w_qkv, q_gain, k_gain, w_o):
    B_, S, _ = h.shape
    qkv = (h @ w_qkv).reshape(B_, S, 3, N_HEADS, HEAD_DIM)
    q = rms_norm(qkv[:, :, 0], q_gain).transpose(0, 2, 1, 3)
    k = rms_norm(qkv[:, :, 1], k_gain).transpose(0, 2, 1, 3)
    v = qkv[:, :, 2].transpose(0, 2, 1, 3)
    return moba_attention(q, k, v) @ w_o


def channel_mlp(h, w_up, w_down):
    a = jax.nn.relu(h @ w_up)
    return (a * a) @ w_down


def setup_inputs(seed: int = 0) -> dict:
    key = jax.random.key(seed)
    ks = jax.random.split(key, 16)
    f32 = jnp.float32
    nrm = lambda k, shape, fan_in: jax.random.normal(k, shape, f32) * (fan_in ** -0.5)
    gain = lambda k, shape: 1.0 + 0.02 * jax.random.normal(k, shape, f32)
    return {
        "x": jax.random.normal(ks[0], (BATCH, SEQ, D_MODEL), f32),
        "mix_norm": gain(ks[1], (DEPTH, D_MODEL)),
        "ffn_norm": gain(ks[2], (DEPTH, D_MODEL)),
        "w_in": nrm(ks[3], (N_EVEN, D_MODEL, MIX_IN_WIDTH), D_MODEL),
        "conv_w": nrm(ks[4], (N_EVEN, CONV_WIDTH, CONV_K), CONV_K),
        "sgu_gain": gain(ks[5], (N_EVEN, SGU_WIDTH)),
        "w_s": nrm(ks[6], (N_EVEN, SGU_GROUPS, SGU_CHUNK, SGU_CHUNK), SGU_CHUNK),
        "b_s": 1.0 + 0.02 * jax.random.normal(ks[7], (N_EVEN, SGU_GROUPS, SGU_CHUNK), f32),
        "w_mix_out": nrm(ks[8], (N_EVEN, D_MODEL, D_MODEL), D_MODEL),
        "w_qkv": nrm(ks[9], (N_ODD, D_MODEL, 3 * D_MODEL), D_MODEL),
        "q_gain": gain(ks[10], (N_ODD, HEAD_DIM)),
        "k_gain": gain(ks[11], (N_ODD, HEAD_DIM)),
        "w_attn_out": nrm(ks[12], (N_ODD, D_MODEL, D_MODEL), D_MODEL),
        "w_up": nrm(ks[13], (DEPTH, D_MODEL, D_FF), D_MODEL),
        "w_down": nrm(ks[14], (DEPTH, D_FF, D_MODEL), D_FF),
    }


def reference(x, mix_norm, ffn_norm, w_in, conv_w, sgu_gain, w_s, b_s, w_mix_out,
              w_qkv, q_gain, k_gain, w_attn_out, w_up, w_down):
    for layer in range(DEPTH):
        i = layer // 2
        h = rms_norm(x, mix_norm[layer])
        if layer % 2 == 0:
            x = x + conv_sgu_mixer(h, w_in[i], conv_w[i], sgu_gain[i], w_s[i], b_s[i], w_mix_out[i])
        else:
            x = x + moba_mixer(h, w_qkv[i], q_gain[i], k_gain[i], w_attn_out[i])
        h = rms_norm(x, ffn_norm[layer])
        x = x + channel_mlp(h, w_up[layer], w_down[layer])
    return x
```

```python
import numpy as np
from contextlib import ExitStack
import concourse.bass as bass
import concourse.mybir as mybir
from concourse.bass_utils import run_bass_kernel_spmd

F32 = mybir.dt.float32
BF16 = mybir.dt.bfloat16
AF = mybir.ActivationFunctionType
ALU = mybir.AluOpType
AX = mybir.AxisListType
U32 = mybir.dt.uint32

D = 2048
S = 16384
NB = 2
TOK = 4096
TT = 512
NTILE = TOK // TT
EPS = 1e-6
NEG = -1.0e30


_UID = [0]


def _uname(n):
    _UID[0] += 1
    return "%s_%d" % (n, _UID[0])


class Buf:
    __slots__ = ("name", "w", "r")

    def __init__(self, name):
        self.name = name
        self.w = None
        self.r = {}


class Prog:
    ENG = ("pe", "act", "dve", "pool", "sp")

    def __init__(self, nc, es):
        self.nc = nc
        self.es = es
        self.ops = {e: [] for e in self.ENG}
        self.sems = {}
        self.cnt = {}
        self.seen = {e: {} for e in self.ENG}
        for e in self.ENG[:4]:
            self._sem(e)

    def _sem(self, key):
        if key not in self.sems:
            self.sems[key] = self.es.enter_context(self.nc.semaphore("s_" + key))
            self.cnt[key] = 0
        return self.sems[key]

    def _deps(self, eng, reads, writes):
        need = {}
        for b in reads:
            if b.w is not None:
                s, v = b.w
                if v > need.get(s, 0):
                    need[s] = v
        for b in writes:
            if b.w is not None:
                s, v = b.w
                if v > need.get(s, 0):
                    need[s] = v
            for s, v in b.r.items():
                if v > need.get(s, 0):
                    need[s] = v
        waits = []
        for s, v in need.items():
            if s == "pe" and eng == "pe":
                continue
            if self.seen[eng].get(s, 0) < v:
                self.seen[eng][s] = v
                waits.append((s, v))
        return waits

    def _record(self, ev, reads, writes):
        s, v = ev
        for b in reads:
            if b.r.get(s, 0) < v:
                b.r[s] = v
        for b in writes:
            b.w = ev
            b.r = {}

    def op(self, eng, fn, reads=(), writes=(), inc=True):
        waits = self._deps(eng, reads, writes)
        if inc:
            self.cnt[eng] += 1
            ev = (eng, self.cnt[eng])
        else:
            ev = (eng, self.cnt[eng] + 1)
        self.ops[eng].append((waits, fn, (eng, 1) if inc else None))
        self._record(ev, reads, writes)

    def dma(self, q, semkey, fn, reads=(), writes=(), amt=16):
        waits = self._deps(q, reads, writes)
        self._sem(semkey)
        self.cnt[semkey] += amt
        ev = (semkey, self.cnt[semkey])
        self.ops[q].append((waits, fn, (semkey, amt)))
        self._record(ev, reads, writes)

    def wait_bufs(self, eng, bufs):
        waits = self._deps(eng, bufs, bufs)
        self.ops[eng].append((waits, None, None))

    def wait_all_dma(self, eng):
        waits = []
        for k, v in self.cnt.items():
            if k not in self.ENG and v > 0 and self.seen[eng].get(k, 0) < v:
                self.seen[eng][k] = v
                waits.append((k, v))
        self.ops[eng].append((waits, None, None))

    @staticmethod
    def merge(dst, srcs):
        for s_ in srcs:
            if s_.w is not None:
                s, v = s_.w
                if dst.r.get(s, 0) < v:
                    dst.r[s] = v
            for s, v in s_.r.items():
                if dst.r.get(s, 0) < v:
                    dst.r[s] = v

    def check(self):
        ptr = {e: 0 for e in self.ENG}
        val = {k: 0 for k in self.sems}
        prog = True
        while prog:
            prog = False
            for e in self.ENG:
                lst = self.ops[e]
                while ptr[e] < len(lst):
                    waits, fn, inc = lst[ptr[e]]
                    if any(val[s] < v for s, v in waits):
                        break
                    if inc is not None:
                        val[inc[0]] += inc[1]
                    ptr[e] += 1
                    prog = True
        for e in self.ENG:
            if ptr[e] < len(self.ops[e]):
                waits = self.ops[e][ptr[e]][0]
                raise RuntimeError("DEADLOCK: engine %s stuck at op %d waits=%s vals=%s" % (
                    e, ptr[e], waits, {s: val[s] for s, _ in waits}))

    def _emit(self, e, eng):
        for waits, fn, inc in self.ops[eng]:
            for s, v in waits:
                e.wait_ge(self.sems[s], v)
            if fn is not None:
                ins = fn(e)
                if inc is not None:
                    ins.then_inc(self.sems[inc[0]], inc[1])

    def barrier(self):
        for e in self.ENG:
            waits = []
            for k, v in self.cnt.items():
                if v > 0 and self.seen[e].get(k, 0) < v:
                    self.seen[e][k] = v
                    waits.append((k, v))
            self.ops[e].append((waits, None, None))

    def emit(self):
        if not hasattr(self, "all_ops"):
            self.all_ops = {e: [] for e in self.ENG}
        cur = self.ops
        for e in self.ENG:
            self.all_ops[e] += cur[e]
        self.ops = self.all_ops
        self.check()
        self.ops = cur
        self._emit_block()
        self.ops = {e: [] for e in self.ENG}

    def _emit_block(self):
        with self.nc.Block() as block:
            @block.tensor
            def _(e):
                self._emit(e, "pe")

            @block.scalar
            def _(e):
                self._emit(e, "act")

            @block.vector
            def _(e):
                self._emit(e, "dve")

            @block.gpsimd
            def _(e):
                self._emit(e, "pool")

            @block.sync
            def _(e):
                self._emit(e, "sp")


class TokBuilder:
    def __init__(self, nc, es, p, nblk, wsrc, wbf, tile_blocks, b_wbf=None):
        self.nc, self.es = nc, es
        self.p = p
        self.wsrc, self.wbf = wsrc, wbf
        self.nblk = nblk
        sb = lambda n, s, d: es.enter_context(nc.sbuf_tensor(_uname("sb_" + n), s, d))
        ps = lambda n, d=F32: es.enter_context(nc.psum_tensor(_uname(n), [128, 512], d))
        self.xt = sb("xt", [128, 16, TT], F32)
        self.b_x = [Buf("x%d" % i) for i in range(16)]
        self.ht = sb("ht", [128, 16, TT], BF16)
        self.b_h = Buf("h")
        self.a = sb("a", [128, 64, TT], BF16)
        self.b_a = [Buf("a%d" % i) for i in range(4)]
        self.cat = self.a[:, 0:16, :]
        self.b_cat = self.b_a[0]
        self.wslot = [sb("w%d" % i, [128, 8192], BF16) for i in range(3)]
        self.b_w = [Buf("w%d" % i) for i in range(3)]
        self.sq = [sb("sq%d" % i, [128, TT], BF16) for i in range(2)]
        self.b_sq = [Buf("sq%d" % i) for i in range(2)]
        self.rt = sb("rt", [128, TT], F32)
        self.b_rt = Buf("rt")
        self.rstd = sb("rstd", [128, TT], F32)
        self.b_rstd = Buf("rstd")
        self.ar = sb("arena", [128, 6208], F32)
        self.ones_bf = sb("ones_bf", [128, 128], BF16)
        self.epst = sb("epst", [128, 1], F32)
        self.b_const = Buf("const")
        self.mm = [ps("mm%d" % i) for i in range(4)]
        self.b_mm = [Buf("mm%d" % i) for i in range(4)]
        self.mmi = 0
        self.ps_n = ps("psn")
        self.b_psn = Buf("psn")
        self.aux = [ps("aux%d" % i) for i in range(3)]
        self.b_aux = [Buf("aux%d" % i) for i in range(3)]
        p.op("pool", lambda e: e.memset(self.ones_bf[:], 1.0), writes=[self.b_const])
        p.op("pool", lambda e: e.memset(self.epst[:], EPS), writes=[self.b_const])
        if b_wbf is None:
            self.b_wbf = [Buf("wbf%d" % i) for i in range(nblk)]
            for i in range(nblk):
                p.dma("pool", "cv%d" % i, lambda e, i=i: e.dma_start(out=wbf[i], in_=wsrc[i]), writes=[self.b_wbf[i]])
        else:
            self.b_wbf = b_wbf
        self.seq = []
        for t in range(NTILE):
            self.seq += tile_blocks
        self.g = 0
        self._wdma(0)
        self._wdma(1)

    def _wdma(self, g):
        if g >= len(self.seq):
            return
        blk = self.seq[g]
        s = g % 3
        self.p.dma("sp", "wl%d" % s, lambda e, s=s, blk=blk: e.dma_start(out=self.wslot[s][:], in_=self.wbf[blk]),
                   reads=[self.b_wbf[blk]], writes=[self.b_w[s]])

    def next_block(self, kc):
        g = self.g
        self._wdma(g + 2)
        self.g += 1
        s = g % 3
        return self.wslot[s][:].rearrange("p (k c) -> p k c", k=kc), self.b_w[s]

    def bank(self):
        i = self.mmi % 4
        self.mmi += 1
        return self.mm[i], self.b_mm[i]

    def mmul(self, out, lhsT, rhs, start, stop, reads, writes, inc):
        self.p.op("pe", lambda e: e.matmul(out, lhsT=lhsT, rhs=rhs, start=start, stop=stop), reads=reads, writes=writes, inc=inc)

    def act(self, out, in_, func, reads, writes, bias=None, scale=None):
        kw = {}
        if bias is not None:
            kw["bias"] = bias
        if scale is not None:
            kw["scale"] = scale
        self.p.op("act", lambda e: e.activation(out=out, in_=in_, func=func, **kw), reads=reads, writes=writes)

    def tt(self, eng, out, in0, in1, op, reads, writes):
        self.p.op(eng, lambda e: e.tensor_tensor(out=out, in0=in0, in1=in1, op=op), reads=reads, writes=writes)

    def stt(self, out, in0, scalar, in1, op0, op1, reads, writes):
        self.p.op("dve", lambda e: e.scalar_tensor_tensor(out=out, in0=in0, scalar=scalar, in1=in1, op0=op0, op1=op1), reads=reads, writes=writes)

    def ts(self, eng, out, in0, s1, s2, op0, op1, reads, writes):
        if op1 is None:
            self.p.op(eng, lambda e: e.tensor_scalar(out=out, in0=in0, scalar1=s1, scalar2=None, op0=op0), reads=reads, writes=writes)
        else:
            self.p.op(eng, lambda e: e.tensor_scalar(out=out, in0=in0, scalar1=s1, scalar2=s2, op0=op0, op1=op1), reads=reads, writes=writes)

    def copy(self, eng, out, in_, reads, writes):
        if eng == "act":
            self.p.op("act", lambda e: e.copy(out=out, in_=in_), reads=reads, writes=writes)
        else:
            self.p.op(eng, lambda e: e.tensor_copy(out=out, in_=in_), reads=reads, writes=writes)

    def rms_rstd(self, ps_ap, ps_buf, N, inv_n, rt, b_rt, rstd, b_rstd):
        self.act(rt[:, 0:N], ps_ap, AF.Sqrt, [ps_buf, self.b_const], [b_rt], bias=self.epst[:, 0:1], scale=inv_n)
        self.p.op("dve", lambda e: e.reciprocal(out=rstd[:, 0:N], in_=rt[:, 0:N]), reads=[b_rt], writes=[b_rstd])

    def norm(self, xk, xbufs, N, gain, gbuf, hk, hbuf):
        for kc in range(16):
            s, sbf = self.sq[kc % 2], self.b_sq[kc % 2]
            self.act(s[:, 0:N], xk(kc), AF.Square, [xbufs[kc]], [sbf])
            self.mmul(self.ps_n[:, 0:N], self.ones_bf[:], s[:, 0:N], kc == 0, kc == 15, [sbf, self.b_const], [self.b_psn], True)
        self.rms_rstd(self.ps_n[:, 0:N], self.b_psn, N, 1.0 / D, self.rt, self.b_rt, self.rstd, self.b_rstd)
        for kc in range(16):
            self.stt(hk(kc), xk(kc), gain[:, kc:kc + 1], self.rstd[:, 0:N], ALU.mult, ALU.mult,
                     [xbufs[kc], self.b_rstd, gbuf], [hbuf])

    def proj_resid(self, nblocks, src, b_src):
        for b4 in range(nblocks):
            W, bW = self.next_block(16)
            for pos in range(4):
                oc = b4 * 4 + pos
                ps_, bps = self.bank()
                for kc in range(16):
                    self.mmul(ps_[:], W[:, kc, pos * 128:(pos + 1) * 128], src[:, kc, :], kc == 0, kc == 15, [bW, b_src], [bps], kc == 15)
                self.tt("dve", self.xt[:, oc, :], ps_[:], self.xt[:, oc, :], ALU.add, [bps, self.b_x[oc]], [self.b_x[oc]])

    def mlp(self, rtmp, b_rtmp):
        for b16 in range(16):
            W, bW = self.next_block(16)
            for pos in range(4):
                hc = b16 * 4 + pos
                ps_, bps = self.bank()
                for kc in range(16):
                    self.mmul(ps_[:], W[:, kc, pos * 128:(pos + 1) * 128], self.ht[:, kc, :], kc == 0, kc == 15, [bW, self.b_h], [bps], kc == 15)
                r, br = rtmp[hc % 2], b_rtmp[hc % 2]
                self.act(r, ps_[:], AF.Relu, [bps], [br])
                self.tt("pool", self.a[:, hc, :], r, r, ALU.mult, [br], [self.b_a[hc // 16]])
        for oc in range(16):
            W, bW = self.next_block(64)
            ps_, bps = self.bank()
            for kc in range(64):
                self.mmul(ps_[:], W[:, kc, :], self.a[:, kc, :], kc == 0, kc == 63, [bW, self.b_a[kc // 16]], [bps], kc == 63)
            self.tt("dve", self.xt[:, oc, :], ps_[:], self.xt[:, oc, :], ALU.add, [bps, self.b_x[oc]], [self.b_x[oc]])


def _dram(nc, name, shape, dty, kind):
    return nc.dram_tensor(name, shape, dty, kind=kind).ap()


NBLK_A = 58
A_TILE_BLOCKS = list(range(58))
NBLK_C = 36
C_TILE_BLOCKS = list(range(36))


def phase_A(nc, p, T):
    xin, xh, wA, ppd, sgd, bsd, wsd = T["xT"], T["xh"], T["wA"], T["ppA"], T["sg"], T["bs"], T["wsT"]
    x1, qloc, kloc, vloc, kmloc, wbf = T["x1"], T["qloc"], T["kloc"], T["vloc"], T["kmloc"], T["wbfA"]
    qT = qloc.rearrange("(h d) t -> d h t", d=128)
    kT = kloc.rearrange("(h d) t -> d h t", d=128)
    vv4 = vloc.rearrange("(h p) (k d) -> p k h d", p=128, d=128)
    kmo = kmloc.rearrange("(h d) k -> d h k", d=128)
    es = ExitStack()
    with es:
        B = TokBuilder(nc, es, p, NBLK_A, wA, wbf, A_TILE_BLOCKS)
        sb = lambda n, s, d: es.enter_context(nc.sbuf_tensor(_uname("sb_" + n), s, d))
        pp = sb("pp", [128, 74], F32)
        sg = sb("sg", [128, 1024], F32)
        bshl = sb("bshl", [33, 1024], BF16)
        ones33 = sb("ones33", [33, 128], BF16)
        wsb = sb("wsb", [128, 8, 128], BF16)
        bsf = B.ar[0:33, 0:1024]
        bst = B.ar[0:33, 1024:2048]
        bshb = B.ar[0:33, 2048:2560].bitcast(BF16)
        wsf = B.ar[:, 3072:4096]
        wsm = B.ar[:, 4096:5120]
        xht = sb("xht", [128, 16, 2], F32)
        hht = sb("hht", [128, 16, 2], BF16)
        gch = sb("gch", [128, 8, 2], F32)
        carry = sb("carry", [128, 8, 2], F32)
        kmacc = sb("kmacc", [128, 16, 16], F32)
        ss4 = [sb("ss4%d" % i, [128, 4], F32) for i in range(2)]
        rt4 = [sb("rt4%d" % i, [128, 4], F32) for i in range(2)]
        rs4 = [sb("rs4%d" % i, [128, 4], F32) for i in range(2)]
        b_ss4 = [Buf("ss4"), Buf("ss4b")]
        b_rt4 = [Buf("rt4"), Buf("rt4b")]
        b_rs4 = [Buf("rs4"), Buf("rs4b")]
        b_pp, b_sg, b_bs, b_ws, b_xh, b_hh = Buf("pp"), Buf("sg"), Buf("bs"), Buf("ws"), Buf("xh"), Buf("hh")
        b_gch = [Buf("gch%d" % i) for i in range(8)]
        b_carry = [Buf("carry%d" % i) for i in range(8)]
        b_km = Buf("km")
        ar = B.ar
        TS = lambda i: ar[:, i * 512:(i + 1) * 512]
        gct = [TS(0), TS(1)]
        cvt = [TS(2), TS(3)]
        ugt = [TS(4), TS(5)]
        vtmp = [TS(6), TS(7)]
        sqv = TS(8)
        yt = [ar[:, 4608:4608 + 514], ar[:, 5632:5632 + 514]]
        b_T = [Buf("T%d" % i) for i in range(12)]
        b_gct, b_cvt, b_ugt, b_vtmp, b_sqv = b_T[0:2], b_T[2:4], b_T[4:6], b_T[6:8], b_T[8]
        b_yt = [[b_T[9], b_T[10]], [b_T[11]]]
        raw = [TS(0), TS(1)]
        b_raw = b_T[0:2]
        kn32 = [TS(2), TS(3)]
        b_kn32 = b_T[2:4]
        qkst = [ar[:, 2048:3072].bitcast(BF16).rearrange("p (h t) -> p h t", h=4),
                ar[:, 3072:4096].bitcast(BF16).rearrange("p (h t) -> p h t", h=4)]
        b_qkst = [[b_T[4], b_T[5]], [b_T[6], b_T[7]]]
        vst = [ar[:, 4096:5120].bitcast(BF16).rearrange("p (h t) -> p h t", h=4),
               ar[:, 5120:6144].bitcast(BF16).rearrange("p (h t) -> p h t", h=4)]
        b_vst = [[b_T[8], b_T[9]], [b_T[10], b_T[11]]]
        vn = B.a[:, 16:24, :].rearrange("p a b -> p (a b)").rearrange("p (t c) -> p t c", t=4)
        b_vn = B.b_a[1]

        p.dma("sp", "c0", lambda e: e.dma_start(out=pp[:], in_=ppd), writes=[b_pp])
        p.dma("sp", "c1", lambda e: e.dma_start(out=sg[:], in_=sgd), writes=[b_sg])
        p.dma("sp", "c2", lambda e: e.dma_start(out=bsf, in_=bsd), writes=[b_bs])
        p.dma("sp", "c3", lambda e: e.dma_start(out=wsf, in_=wsd), writes=[b_ws])
        p.dma("sp", "c4", lambda e: e.dma_start(out=xht[:], in_=xh), writes=[b_xh])
        p.op("pool", lambda e: e.memset(kmacc[:], 0.0), writes=[b_km])
        p.op("dve", lambda e: e.tensor_copy(out=bshb, in_=bsf), reads=[b_bs], writes=[b_bs])
        p.op("dve", lambda e: e.tensor_copy(out=bst, in_=bshb), reads=[b_bs], writes=[b_bs])
        p.op("dve", lambda e: e.tensor_tensor(out=bst, in0=bsf, in1=bst, op=ALU.subtract), reads=[b_bs], writes=[b_bs])
        p.op("dve", lambda e: e.tensor_copy(out=bshl[:], in_=bshb), reads=[b_bs], writes=[b_bs])
        p.op("dve", lambda e: e.tensor_copy(out=bshl[32:33, :], in_=bst[32:33, :]), reads=[b_bs], writes=[b_bs])
        p.op("pool", lambda e: e.memset(ones33[:], 1.0), writes=[b_bs])
        p.op("pool", lambda e: e.affine_select(out=wsm, in_=wsf, pattern=[[0, 8], [1, 128]], compare_op=ALU.is_ge,
                                                fill=0.0, base=0, channel_multiplier=-1), reads=[b_ws], writes=[b_ws])
        p.op("pool", lambda e: e.tensor_copy(out=wsb[:].rearrange("p g i -> p (g i)"), in_=wsm), reads=[b_ws], writes=[b_ws])
        for bt in b_T:
            Prog.merge(bt, [b_bs, b_ws])

        cw = lambda c, tap: pp[:, 48 + c * 3 + tap:48 + c * 3 + tap + 1]
        qg = pp[:, 72:73]
        kg = pp[:, 73:74]

        B.norm(lambda kc: xht[:, kc, :], [b_xh] * 16, 2, pp[:, 0:16], b_pp, lambda kc: hht[:, kc, :], b_hh)

        b_io = Buf("io")
        outs = [T["b_x1"], T["b_qloc"], T["b_kloc"], T["b_vloc"], T["b_kmloc"]]
        for t in range(NTILE):
            t0 = t * TT
            p.dma("sp", "xl", lambda e, t0=t0: e.dma_start(out=B.xt[:], in_=xin[:, :, t0:t0 + TT]), writes=B.b_x)
            B.norm(lambda kc: B.xt[:, kc, :], B.b_x, TT, pp[:, 0:16], b_pp, lambda kc: B.ht[:, kc, :], B.b_h)
            W = bW = None
            for i in range(24):
                blk, pos = divmod(i, 4)
                c, kind = divmod(i, 3)
                if pos == 0:
                    W, bW = B.next_block(16)
                ps_, bps = B.bank()
                for kc in range(16):
                    B.mmul(ps_[:], W[:, kc, pos * 128:(pos + 1) * 128], B.ht[:, kc, :], kc == 0, kc == 15, [bW, B.b_h], [bps], kc == 15)
                if t == 0 and kind < 2:
                    hp, bhp = B.aux[2], B.b_aux[2]
                    for kc in range(16):
                        B.mmul(hp[:, 0:2], W[:, kc, pos * 128:(pos + 1) * 128], hht[:, kc, :], kc == 0, kc == 15, [bW, b_hh], [bhp], kc == 15)
                    if kind == 0:
                        B.copy("act", gch[:, c, :], hp[:, 0:2], [bhp], [b_gch[c]])
                    else:
                        B.tt("dve", carry[:, c, :], hp[:, 0:2], gch[:, c, :], ALU.mult, [bhp, b_gch[c]], [b_carry[c]])
                if kind == 0:
                    B.copy("act", gct[c % 2], ps_[:], [bps], [b_gct[c % 2]])
                elif kind == 1:
                    y, by = yt[c % 2], b_yt[c % 2]
                    B.tt("dve", y[:, 2:514], ps_[:], gct[c % 2], ALU.mult, [bps, b_gct[c % 2]], by)
                    B.copy("pool", y[:, 0:2], carry[:, c, :], [b_carry[c]], by)
                    B.copy("pool", carry[:, c, :], y[:, 512:514], by, [b_carry[c]])
                    cv, bcv = cvt[c % 2], b_cvt[c % 2]
                    B.ts("dve", cv, y[:, 0:512], cw(c, 0), None, ALU.mult, None, by + [b_pp], [bcv])
                    B.stt(cv, y[:, 1:513], cw(c, 1), cv, ALU.mult, ALU.add, by + [b_pp, bcv], [bcv])
                    B.stt(cv, y[:, 2:514], cw(c, 2), cv, ALU.mult, ALU.add, by + [b_pp, bcv], [bcv])
                else:
                    B.tt("dve", B.cat[:, c, :], ps_[:], cvt[c % 2], ALU.mult, [bps, b_cvt[c % 2]], [B.b_cat])
            for vb in range(2):
                W, bW = B.next_block(16)
                for tc in range(4):
                    i2 = vb * 4 + tc
                    ps_, bps = B.bank()
                    for kc in range(16):
                        B.mmul(ps_[:], B.ht[:, kc, tc * 128:(tc + 1) * 128], W[:, kc, :], kc == 0, kc == 15, [bW, B.b_h], [bps], kc == 15)
                    vt, bvt = vtmp[i2 % 2], b_vtmp[i2 % 2]
                    B.act(vt, ps_[:], AF.Gelu_apprx_tanh, [bps], [bvt])
                    B.tt("pool", sqv, vt, vt, ALU.mult, [bvt], [b_sqv])
                    s4, r4, q4 = ss4[i2 % 2], rt4[i2 % 2], rs4[i2 % 2]
                    p.op("dve", lambda e, s4=s4: e.tensor_reduce(out=s4[:], in_=sqv.rearrange("p (g d) -> p g d", g=4), axis=AX.X, op=ALU.add),
                         reads=[b_sqv], writes=[b_ss4[i2 % 2]])
                    B.act(r4[:], s4[:], AF.Sqrt, [b_ss4[i2 % 2], B.b_const], [b_rt4[i2 % 2]], bias=B.epst[:, 0:1], scale=1.0 / 128)
                    p.op("dve", lambda e, r4=r4, q4=q4: e.reciprocal(out=q4[:], in_=r4[:]), reads=[b_rt4[i2 % 2]], writes=[b_rs4[i2 % 2]])
                    for g4 in range(4):
                        c0 = vb * 512 + g4 * 128
                        B.stt(vn[:, tc, c0:c0 + 128], vt[:, g4 * 128:(g4 + 1) * 128], q4[:, g4:g4 + 1], sg[:, c0:c0 + 128], ALU.mult, ALU.mult,
                              [bvt, b_rs4[i2 % 2], b_sg], [b_vn])
            for ub in range(2):
                W, bW = B.next_block(16)
                for pos in range(4):
                    c = ub * 4 + pos
                    ps_, bps = B.bank()
                    for kc in range(16):
                        B.mmul(ps_[:], W[:, kc, pos * 128:(pos + 1) * 128], B.ht[:, kc, :], kc == 0, kc == 15, [bW, B.b_h], [bps], kc == 15)
                    pm, bpm = B.aux[c % 2], B.b_aux[c % 2]
                    for tc in range(4):
                        B.mmul(pm[:, tc * 128:(tc + 1) * 128], vn[:, tc, c * 128:(c + 1) * 128], wsb[:, c, :], True, False, [b_vn, b_ws], [bpm], False)
                        B.mmul(pm[:, tc * 128:(tc + 1) * 128], ones33[:], bshl[:, c * 128:(c + 1) * 128], False, True, [b_bs], [bpm], tc == 3)
                    B.act(ugt[c % 2], ps_[:], AF.Gelu_apprx_tanh, [bps], [b_ugt[c % 2]])
                    B.tt("dve", B.cat[:, 8 + c, :], pm[:], ugt[c % 2], ALU.mult, [bpm, b_ugt[c % 2]], [B.b_cat])
            B.proj_resid(4, B.cat, B.b_cat)
            B.norm(lambda kc: B.xt[:, kc, :], B.b_x, TT, pp[:, 16:32], b_pp, lambda kc: B.ht[:, kc, :], B.b_h)
            B.mlp(gct, b_gct)
            p.dma("sp", "so_x", lambda e, t0=t0: e.dma_start(out=x1[:, :, t0:t0 + TT], in_=B.xt[:]), reads=B.b_x, writes=[outs[0]])
            B.norm(lambda kc: B.xt[:, kc, :], B.b_x, TT, pp[:, 32:48], b_pp, lambda kc: B.ht[:, kc, :], B.b_h)
            for qb in range(8):
                W, bW = B.next_block(16)
                isk = qb >= 4
                st, bst_ = qkst[qb % 2], b_qkst[qb % 2]
                for pos in range(4):
                    head = (qb % 4) * 4 + pos
                    ps_, bps = B.bank()
                    for kc in range(16):
                        B.mmul(ps_[:], W[:, kc, pos * 128:(pos + 1) * 128], B.ht[:, kc, :], kc == 0, kc == 15, [bW, B.b_h], [bps], kc == 15)
                    rw, brw = raw[pos % 2], b_raw[pos % 2]
                    B.copy("act", rw, ps_[:], [bps], [brw])
                    s, sbf = B.sq[pos % 2], B.b_sq[pos % 2]
                    B.tt("pool", s[:], rw, rw, ALU.mult, [brw], [sbf])
                    pn, bpn = B.aux[pos % 2], B.b_aux[pos % 2]
                    B.mmul(pn[:], B.ones_bf[:], s[:], True, True, [sbf, B.b_const], [bpn], True)
                    B.rms_rstd(pn[:], bpn, TT, 1.0 / 128, B.rt, B.b_rt, B.rstd, B.b_rstd)
                    kn, bkn = kn32[pos % 2], b_kn32[pos % 2]
                    B.stt(kn, rw, kg if isk else qg, B.rstd[:], ALU.mult, ALU.mult, [brw, B.b_rstd, b_pp], [bkn])
                    B.copy("pool", st[:, pos, :], kn, [bkn], bst_)
                    if isk:
                        p.op("dve", lambda e, kn=kn, head=head, t=t: e.tensor_reduce(
                            out=kmacc[:, head, 2 * t:2 * t + 2], in_=kn.rearrange("p (b k) -> p b k", b=2), axis=AX.X, op=ALU.add),
                            reads=[bkn], writes=[b_km])
                dst = kT if isk else qT
                h0 = (qb % 4) * 4
                p.dma("sp", "so_q%d" % (qb % 2), lambda e, dst=dst, h0=h0, st=st, t0=t0: e.dma_start(out=dst[:, h0:h0 + 4, t0:t0 + TT], in_=st),
                      reads=bst_, writes=[outs[2 if isk else 1]])
            for vb in range(4):
                W, bW = B.next_block(16)
                st, bst_ = vst[vb % 2], b_vst[vb % 2]
                for tc in range(4):
                    ps_, bps = B.bank()
                    for kc in range(16):
                        B.mmul(ps_[:], B.ht[:, kc, tc * 128:(tc + 1) * 128], W[:, kc, :], kc == 0, kc == 15, [bW, B.b_h], [bps], kc == 15)
                    B.copy("act", st[:, tc, :], ps_[:], [bps], bst_)
                for hh in range(4):
                    p.dma("sp", "so_v%d" % (vb % 2), lambda e, st=st, vb=vb, t=t, hh=hh: e.dma_start(
                        out=vv4[:, 4 * t:4 * t + 4, 4 * vb + hh, :], in_=st[:, :, hh * 128:(hh + 1) * 128]),
                        reads=bst_, writes=[outs[3]])
            if t == 0 and T.get("convC") is not None:
                T["convC"]()
        p.op("act", lambda e: e.mul(out=kmacc[:], in_=kmacc[:], mul=1.0 / 256), reads=[b_km], writes=[b_km])
        p.dma("sp", "so_km", lambda e: e.dma_start(out=kmo, in_=kmacc[:]), reads=[b_km], writes=[outs[4]])
        T["gatherA"]()
        p.barrier()
        p.emit()


def phase_C(nc, p, T):
    x1, og, wbf, ppd, xo, idxd = T["x1"], T["og"], T["wbfC"], T["ppC"], T["xo"], T["idxC"]
    es = ExitStack()
    with es:
        B = TokBuilder(nc, es, p, NBLK_C, None, wbf, C_TILE_BLOCKS, b_wbf=[T["b_wbfC"]] * NBLK_C)
        pp = es.enter_context(nc.sbuf_tensor("sb_ppc", [128, 16], F32))
        idx = es.enter_context(nc.sbuf_tensor("sb_idxc", [128, 128], U32))
        b_pp = Buf("pp")
        p.dma("sp", "c0", lambda e: e.dma_start(out=pp[:], in_=ppd), writes=[b_pp])
        p.dma("sp", "c1", lambda e: e.dma_start(out=idx[:], in_=idxd), writes=[b_pp])
        rtmp = [B.ar[:, 0:512], B.ar[:, 512:1024]]
        b_rtmp = [Buf("r0"), Buf("r1")]
        b_out = Buf("out")
        for t in range(NTILE):
            t0 = t * TT
            p.dma("sp", "xl", lambda e, t0=t0: e.dma_start(out=B.xt[:], in_=x1[:, :, t0:t0 + TT]), reads=[T["b_x1"]], writes=B.b_x)
            for h in range(16):
                p.dma("pool", "al%d" % (h % 2), lambda e, h=h, t=t: e.indirect_dma_start(
                    out=B.cat[:, h, :], out_offset=None, in_=og,
                    in_offset=bass.IndirectOffsetOnAxis(ap=idx[:, t * 16 + h:t * 16 + h + 1], axis=0)),
                    reads=[T["b_og"], b_pp], writes=[B.b_cat])
            B.proj_resid(4, B.cat, B.b_cat)
            B.norm(lambda kc: B.xt[:, kc, :], B.b_x, TT, pp[:, 0:16], b_pp, lambda kc: B.ht[:, kc, :], B.b_h)
            B.mlp(rtmp, b_rtmp)
            p.dma("sp", "so_x", lambda e, t0=t0: e.dma_start(out=xo[:, :, t0:t0 + TT], in_=B.xt[:]), reads=B.b_x, writes=[b_out])
        p.wait_all_dma("sp")
        p.barrier()
        p.emit()


NPAIR = 4
SCALE = 128 ** -0.5
BIGS = 30000.0 / SCALE


def phase_B(nc, p, T, npair=NPAIR, nqt=S // TT):
    qg, kg, vg, kmg, sld, oloc, idxd = T["qg"], T["kg"], T["vg"], T["kmg"], T["slope"], T["oloc"], T["idxB"]
    es = ExitStack()
    with es:
        sb = lambda n, s, d: es.enter_context(nc.sbuf_tensor(_uname("sb_" + n), s, d))
        psm = lambda n, d=F32: es.enter_context(nc.psum_tensor(_uname(n), [128, 512], d))
        qT = sb("qT", [128, S], BF16)
        kT = sb("kT", [128, S], BF16)
        vt = sb("vt", [128, 128, 128], BF16)
        kmf = sb("kmf", [128, 64], F32)
        kmb = sb("kmb", [128, 64], BF16)
        slope = sb("slope", [128, npair], F32)
        idx = sb("idxb", [128, 16], U32)
        ident = sb("ident", [128, 128], BF16)
        ones32 = sb("ones32", [128, 128], F32)
        E = sb("E", [65, 65, 128], BF16)
        iot = sb("iot", [128, 131], F32)
        kb = sb("kb", [128, npair, 131], F32)
        iot4 = sb("iot4", [128, 4], F32)
        qterm = sb("qterm", [128, npair, 4], F32)
        gm = [sb("gm%d" % i, [128, 64], F32) for i in range(2)]
        mx = [sb("mx%d" % i, [128, 8], F32) for i in range(2)]
        nm32 = [sb("nm32%d" % i, [128, 65], F32) for i in range(2)]
        nmb = [sb("nmb%d" % i, [128, 65], BF16) for i in range(8)]
        NMT = [sb("NMT%d" % i, [65, 512], BF16) for i in range(2)]
        pT = [sb("pT%d" % i, [128, 512], BF16) for i in range(3)]
        acc = sb("acc", [128, 512], F32)
        rl = sb("rl", [128, 512], F32)
        ot = [sb("ot%d" % i, [128, 512], BF16) for i in range(2)]
        ps_s = [psm("ps_s%d" % i) for i in range(3)]
        ps_o = [psm("ps_o%d" % i) for i in range(2)]
        ps_l = psm("ps_l")
        pg = psm("pg")
        pt = psm("pt", BF16)
        b_q, b_k, b_v, b_km, b_c = Buf("q"), Buf("k"), Buf("v"), Buf("km"), Buf("c")
        b_gm = [Buf("gm0"), Buf("gm1")]
        b_mx = [Buf("mx0"), Buf("mx1")]
        b_nm32 = [Buf("nm320"), Buf("nm321")]
        b_nmb = [Buf("nmb%d" % i) for i in range(8)]
        b_NMT = [Buf("NMT0"), Buf("NMT1")]
        b_pT = [Buf("pT%d" % i) for i in range(3)]
        b_acc, b_rl = Buf("acc"), Buf("rl")
        b_ot = [Buf("ot0"), Buf("ot1")]
        b_ps_s = [Buf("pss%d" % i) for i in range(3)]
        b_ps_o = [Buf("pso%d" % i) for i in range(2)]
        b_ps_l, b_pg, b_pt = Buf("psl"), Buf("pg"), Buf("pt")
        b_out = Buf("out")

        p.dma("sp", "c0", lambda e: e.dma_start(out=slope[:], in_=sld), writes=[b_c])
        p.dma("sp", "c1", lambda e: e.dma_start(out=idx[:], in_=idxd), writes=[b_c])
        p.op("pool", lambda e: e.memset(ident[:], 1.0), writes=[b_c])
        p.op("pool", lambda e: e.affine_select(out=ident[:], in_=ident[:], pattern=[[1, 128]], compare_op=ALU.is_equal, fill=0.0,
                                                base=0, channel_multiplier=-1), reads=[b_c], writes=[b_c])
        p.op("pool", lambda e: e.memset(ones32[:], 1.0), writes=[b_c])
        p.op("pool", lambda e: e.memset(E[:], 1.0), writes=[b_c])
        p.op("pool", lambda e: e.affine_select(out=E[:], in_=E[:], pattern=[[1, 65], [0, 128]], compare_op=ALU.is_equal, fill=0.0,
                                                base=0, channel_multiplier=-1), reads=[b_c], writes=[b_c])
        p.op("pool", lambda e: e.iota(iot[:], pattern=[[128, 131]], base=-127 * 128, channel_multiplier=1,
                                       allow_small_or_imprecise_dtypes=True), writes=[b_c])
        p.op("pool", lambda e: e.iota(iot4[:], pattern=[[128, 4]], base=0, channel_multiplier=1,
                                       allow_small_or_imprecise_dtypes=True), writes=[b_c])
        for i in range(npair):
            p.op("dve", lambda e, i=i: e.tensor_scalar(out=kb[:, i, :], in0=iot[:], scalar1=slope[:, i:i + 1], scalar2=None, op0=ALU.mult),
                 reads=[b_c], writes=[b_c])
            p.op("dve", lambda e, i=i: e.tensor_scalar(out=qterm[:, i, :], in0=iot4[:], scalar1=slope[:, i:i + 1], scalar2=-1.0 / SCALE,
                                                        op0=ALU.mult, op1=ALU.mult), reads=[b_c], writes=[b_c])
        for i in range(2):
            p.op("pool", lambda e, i=i: e.memset(nm32[i][:], 0.0), writes=[b_nm32[i]])

        def gateA(pi, qt):
            par = qt % 2
            for j in range(4):
                own = 2 * qt + j // 2
                g2 = j % 2
                qs = qT[:, qt * TT + j * 128: qt * TT + (j + 1) * 128]
                p.op("pe", lambda e, qs=qs: e.matmul(pg[:, 0:64], lhsT=qs, rhs=kmb[:], start=True, stop=True), reads=[b_q, b_km], writes=[b_pg])
                if own > 0:
                    p.op("act", lambda e, g2=g2, own=own: e.copy(out=gm[g2][:, 0:own], in_=pg[:, 0:own]), reads=[b_pg], writes=[b_gm[g2]])
                p.op("dve", lambda e, g2=g2: e.max(out=mx[g2][:], in_=gm[g2][:]), reads=[b_gm[g2]], writes=[b_mx[g2]])
                p.op("dve", lambda e, g2=g2: e.tensor_scalar(out=nm32[g2][:, 0:64], in0=gm[g2][:], scalar1=mx[g2][:, 2:3], scalar2=-BIGS,
                                                             op0=ALU.is_lt, op1=ALU.mult), reads=[b_gm[g2], b_mx[g2]], writes=[b_nm32[g2]])
                nb_ = nmb[par * 4 + j]
                p.op("dve", lambda e, g2=g2, nb_=nb_, j=j: e.tensor_scalar(out=nb_[:], in0=nm32[g2][:], scalar1=qterm[:, pi, j:j + 1], scalar2=None,
                                                                          op0=ALU.add), reads=[b_nm32[g2], b_c], writes=[b_nmb[par * 4 + j]])

        def gateB(pi, qt):
            par = qt % 2
            for j in range(4):
                nb_ = nmb[par * 4 + j]
                p.op("pe", lambda e, nb_=nb_, j=j: e.transpose(out=pt[0:65, j * 128:(j + 1) * 128], in_=nb_[:], identity=ident[:]),
                     reads=[b_nmb[par * 4 + j], b_c], writes=[b_pt])
            p.op("act", lambda e: e.copy(out=NMT[par][:], in_=pt[0:65, :]), reads=[b_pt], writes=[b_NMT[par]])

        step = [0]

        def keyloop(pi, qt):
            par = qt % 2
            po, bpo = ps_o[par], b_ps_o[par]
            nkt = 4 * qt + 4
            for kt in range(nkt):
                n = kt // 2
                iin = kt - 4 * qt
                if iin < 0:
                    ranges = [(0, 512, n)]
                elif iin == 0:
                    ranges = [(0, 256, 64), (256, 512, n)]
                elif iin == 1:
                    ranges = [(128, 256, 64), (256, 512, n)]
                elif iin == 2:
                    ranges = [(256, 512, 64)]
                else:
                    ranges = [(384, 512, 64)]
                c0 = ranges[0][0]
                si = step[0] % 3
                step[0] += 1
                pss, bpss = ps_s[si], b_ps_s[si]
                pTt, bpT = pT[si], b_pT[si]
                q0 = qt * TT
                p.op("pe", lambda e, pss=pss, kt=kt, c0=c0, q0=q0: e.matmul(pss[:, c0:512], lhsT=kT[:, kt * 128:(kt + 1) * 128],
                                                                         rhs=qT[:, q0 + c0:q0 + 512], start=True, stop=False),
                     reads=[b_k, b_q], writes=[bpss], inc=False)
                for ri, (a_, b_, row) in enumerate(ranges):
                    last = ri == len(ranges) - 1
                    p.op("pe", lambda e, pss=pss, a_=a_, b_=b_, row=row, last=last: e.matmul(
                        pss[:, a_:b_], lhsT=E[:, row, :], rhs=NMT[par][:, a_:b_], start=False, stop=last),
                        reads=[b_c, b_NMT[par]], writes=[bpss], inc=last)
                mi = kt - 4 * qt + 127
                p.op("act", lambda e, pss=pss, pTt=pTt, c0=c0, mi=mi: e.activation(out=pTt[:, c0:512], in_=pss[:, c0:512], func=AF.Exp,
                                                                                 bias=kb[:, pi, mi:mi + 1], scale=SCALE),
                     reads=[bpss, b_c], writes=[bpT])
                if iin >= 0:
                    j = iin
                    p.op("pool", lambda e, pTt=pTt, j=j: e.affine_select(out=pTt[:, j * 128:(j + 1) * 128], in_=pTt[:, j * 128:(j + 1) * 128],
                                                                       pattern=[[1, 128]], compare_op=ALU.is_ge, fill=0.0, base=0,
                                                                       channel_multiplier=-1), reads=[bpT], writes=[bpT])
                if kt == 0:
                    p.op("dve", lambda e, pTt=pTt: e.tensor_copy(out=acc[:], in_=pTt[:]), reads=[bpT], writes=[b_acc])
                else:
                    p.op("dve", lambda e, pTt=pTt, c0=c0: e.tensor_tensor(out=acc[:, c0:512], in0=acc[:, c0:512], in1=pTt[:, c0:512], op=ALU.add),
                         reads=[bpT, b_acc], writes=[b_acc])
                p.op("pe", lambda e, po=po, kt=kt, c0=c0, pTt=pTt, nkt=nkt: e.matmul(po[:, c0:512], lhsT=vt[:, kt, :], rhs=pTt[:, c0:512],
                                                                                   start=(kt == 0), stop=(kt == nkt - 1)),
                     reads=[b_v, bpT], writes=[bpo], inc=True)
            p.op("pe", lambda e: e.matmul(ps_l[:], lhsT=ones32[:], rhs=acc[:], start=True, stop=True), reads=[b_acc, b_c], writes=[b_ps_l])
            p.op("dve", lambda e: e.reciprocal(out=rl[:], in_=ps_l[:]), reads=[b_ps_l], writes=[b_rl])
            o_, bo = ot[par], b_ot[par]
            p.op("dve", lambda e, o_=o_, po=po: e.tensor_tensor(out=o_[:], in0=po[:], in1=rl[:], op=ALU.mult), reads=[bpo, b_rl], writes=[bo])
            row0 = (pi * 32 + qt) * 128
            p.dma("sp", "so%d" % par, lambda e, o_=o_, row0=row0: e.dma_start(out=oloc[row0:row0 + 128, :], in_=o_[:]), reads=[bo], writes=[T["b_oloc"]])

        for pi in range(npair):
            vflat = vt[:].rearrange("p t d -> p (t d)")
            for r in range(4):
                io = bass.IndirectOffsetOnAxis(ap=idx[:, pi * 4 + r:pi * 4 + r + 1], axis=0)
                first = r == 0
                p.dma("pool", "lq", lambda e, r=r, io=io: e.indirect_dma_start(out=qT[:, r * TOK:(r + 1) * TOK], out_offset=None, in_=qg, in_offset=io),
                      reads=[T["b_qg"], b_c], writes=[b_q] if first else [])
                p.dma("pool", "lk", lambda e, r=r, io=io: e.indirect_dma_start(out=kT[:, r * TOK:(r + 1) * TOK], out_offset=None, in_=kg, in_offset=io),
                      reads=[T["b_kg"], b_c], writes=[b_k] if first else [])
                p.dma("pool", "lv", lambda e, r=r, io=io: e.indirect_dma_start(out=vflat[:, r * TOK:(r + 1) * TOK], out_offset=None, in_=vg, in_offset=io),
                      reads=[T["b_vg"], b_c], writes=[b_v] if first else [])
                p.dma("pool", "lm", lambda e, r=r, io=io: e.indirect_dma_start(out=kmf[:, r * 16:(r + 1) * 16], out_offset=None, in_=kmg, in_offset=io),
                      reads=[T["b_kmg"], b_c], writes=[b_km] if first else [])
            b_q.w, b_k.w, b_v.w, b_km.w = ("lq", p.cnt["lq"]), ("lk", p.cnt["lk"]), ("lv", p.cnt["lv"]), ("lm", p.cnt["lm"])
            p.op("dve", lambda e: e.tensor_copy(out=kmb[:], in_=kmf[:]), reads=[b_km], writes=[b_km])
            for i in range(2):
                p.op("pool", lambda e, i=i: e.memset(gm[i][:], NEG), writes=[b_gm[i]])
            gateA(pi, 0)
            gateB(pi, 0)
            for qt in range(nqt):
                if qt + 1 < nqt:
                    gateA(pi, qt + 1)
                keyloop(pi, qt)
                if qt + 1 < nqt:
                    gateB(pi, qt + 1)
        T["gatherB"]()
        p.barrier()
        p.emit()


def _tile16(W, n0, ncols=512):
    return np.ascontiguousarray(W[:, n0:n0 + ncols].reshape(16, 128, ncols).transpose(1, 0, 2)).reshape(128, 16 * ncols)


def _tile64(W, n0):
    return np.ascontiguousarray(W[:, n0:n0 + 128].reshape(64, 128, 128).transpose(1, 0, 2)).reshape(128, 8192)


def _fm(v):
    return np.ascontiguousarray(v.reshape(16, 128).T)


def _slopes():
    return (2.0 ** (-8.0 * np.arange(1, 17, dtype=np.float32) / 16)).astype(np.float32)


_CACHE = {}


def _get(name, fn):
    if name not in _CACHE:
        _CACHE[name] = fn()
    return _CACHE[name]


def prep_A(inp):
    x = inp["x"]
    w_in = inp["w_in"][0]
    order = []
    for c in range(8):
        order += list(range(1024 + 128 * c, 1024 + 128 * (c + 1)))
        order += list(range(2048 + 128 * c, 2048 + 128 * (c + 1)))
        order += list(range(0 + 128 * c, 0 + 128 * (c + 1)))
    order += list(range(4096, 5120))
    order += list(range(3072, 4096))
    w_in_p = w_in[:, order]
    blocks = [_tile16(w_in_p, n0) for n0 in range(0, 5120, 512)]
    blocks += [_tile16(inp["w_mix_out"][0], n0) for n0 in range(0, 2048, 512)]
    blocks += [_tile16(inp["w_up"][0], n0) for n0 in range(0, 8192, 512)]
    blocks += [_tile64(inp["w_down"][0], n0) for n0 in range(0, 2048, 128)]
    blocks += [_tile16(inp["w_qkv"][0], n0) for n0 in range(0, 6144, 512)]
    wA = np.stack(blocks).astype(np.float32)
    assert wA.shape == (NBLK_A, 128, 8192)
    pp = np.zeros((128, 74), np.float32)
    pp[:, 0:16] = _fm(inp["mix_norm"][0])
    pp[:, 16:32] = _fm(inp["ffn_norm"][0])
    pp[:, 32:48] = _fm(inp["mix_norm"][1])
    pp[:, 48:72] = inp["conv_w"][0].reshape(8, 128, 3).transpose(1, 0, 2).reshape(128, 24)
    pp[:, 72] = inp["q_gain"][0]
    pp[:, 73] = inp["k_gain"][0]
    sg = np.ascontiguousarray(np.broadcast_to(inp["sgu_gain"][0][None, :], (128, 1024))).astype(np.float32)
    bs = np.zeros((33, 1024), np.float32)
    bs[0] = inp["b_s"][0].reshape(1024)
    bs[32] = inp["b_s"][0].reshape(1024)
    wsT = np.ascontiguousarray(inp["w_s"][0].transpose(2, 0, 1)).reshape(128, 1024).astype(np.float32)
    maps = []
    for c in range(8):
        b, r = divmod(c, 4)
        s0 = r * TOK
        xs = x[b, s0:s0 + TOK]
        xT = np.ascontiguousarray(xs.reshape(TOK, 16, 128).transpose(2, 1, 0))
        xh = np.zeros((128, 16, 2), np.float32)
        if r > 0:
            xh[:] = x[b, s0 - 2:s0].reshape(2, 16, 128).transpose(2, 1, 0)
        maps.append({"xT": xT, "xh": xh, "wA": wA, "pp": pp, "sg": sg, "bs": bs, "wsT": wsT})
    return maps


def build_F():
    nc = bass.Bass("TRN2", target_bir_lowering=False)
    T = {}
    ext = lambda n, sh, dt: _dram(nc, n, sh, dt, "ExternalInput")
    itn = lambda n, sh, dt: _dram(nc, n, sh, dt, "Internal")
    T["xT"] = ext("xT", [128, 16, TOK], F32)
    T["xh"] = ext("xh", [128, 16, 2], F32)
    T["wA"] = ext("wA", [NBLK_A, 128, 8192], F32)
    T["wC"] = ext("wC", [NBLK_C, 128, 8192], F32)
    T["ppA"] = ext("pp", [128, 74], F32)
    T["ppC"] = ext("ppC", [128, 16], F32)
    T["sg"] = ext("sg", [128, 1024], F32)
    T["bs"] = ext("bs", [33, 1024], F32)
    T["wsT"] = ext("wsT", [128, 1024], F32)
    T["slope"] = ext("slope", [128, NPAIR], F32)
    T["idxB"] = ext("idxB", [128, 16], U32)
    T["idxC"] = ext("idxC", [128, 128], U32)
    T["xo"] = _dram(nc, "xo", [128, 16, TOK], F32, "ExternalOutput")
    T["wbfA"] = itn("wbfA", [NBLK_A, 128, 8192], BF16)
    T["wbfC"] = itn("wbfC", [NBLK_C, 128, 8192], BF16)
    T["x1"] = itn("x1", [128, 16, TOK], F32)
    for nm in ("q", "k", "v"):
        T[nm + "loc"] = itn(nm + "loc", [2048, TOK], BF16)
        T[nm + "g"] = itn(nm + "g", [4 * 2048, TOK], BF16)
    T["kmloc"] = itn("kmloc", [2048, 16], F32)
    T["kmg"] = itn("kmg", [4 * 2048, 16], F32)
    T["oloc"] = itn("oloc", [NPAIR * 32 * 128, TT], BF16)
    T["og"] = itn("og", [4 * NPAIR * 32 * 128, TT], BF16)
    for nm in ("x1", "qloc", "kloc", "vloc", "kmloc", "qg", "kg", "vg", "kmg", "oloc", "og", "wbfC"):
        T["b_" + nm] = Buf(nm)
    groups = [[0, 1, 2, 3], [4, 5, 6, 7]]
    es = ExitStack()
    with es:
        p = Prog(nc, es)

        def convC():
            for i in range(NBLK_C):
                p.dma("pool", "cvC", lambda e, i=i: e.dma_start(out=T["wbfC"][i], in_=T["wC"][i]))
            T["b_wbfC"].w = ("cvC", p.cnt["cvC"])

        def gatherA():
            for nm in ("q", "k", "v"):
                for h in range(16):
                    p.dma("pool", "cc" + nm, lambda e, nm=nm, h=h: e.collective_compute(
                        "AllGather", ALU.bypass, replica_groups=groups,
                        ins=[T[nm + "loc"][h * 128:(h + 1) * 128, :].opt()], outs=[T[nm + "g"][h * 512:(h + 1) * 512, :].opt()]),
                        reads=[T["b_" + nm + "loc"]], writes=[T["b_" + nm + "g"]], amt=1)
            for h in range(16):
                p.dma("pool", "cckm", lambda e, h=h: e.collective_compute(
                    "AllGather", ALU.bypass, replica_groups=groups,
                    ins=[T["kmloc"][h * 128:(h + 1) * 128, :].opt()], outs=[T["kmg"][h * 512:(h + 1) * 512, :].opt()]),
                    reads=[T["b_kmloc"]], writes=[T["b_kmg"]], amt=1)

        def gatherB():
            for c in range(16):
                p.dma("pool", "cco", lambda e, c=c: e.collective_compute(
                    "AllGather", ALU.bypass, replica_groups=groups,
                    ins=[T["oloc"][c * 1024:(c + 1) * 1024, :].opt()], outs=[T["og"][c * 4096:(c + 1) * 4096, :].opt()]),
                    reads=[T["b_oloc"]], writes=[T["b_og"]], amt=1)

        T["convC"], T["gatherA"], T["gatherB"] = convC, gatherA, gatherB
        phase_A(nc, p, T)
        phase_B(nc, p, T)
        phase_C(nc, p, T)
    return nc


def prep_F(inp):
    maps = prep_A(inp)
    blocks = [_tile16(inp["w_attn_out"][0], n0) for n0 in range(0, 2048, 512)]
    blocks += [_tile16(inp["w_up"][1], n0) for n0 in range(0, 8192, 512)]
    blocks += [_tile64(inp["w_down"][1], n0) for n0 in range(0, 2048, 128)]
    wC = np.stack(blocks).astype(np.float32)
    ppC = _fm(inp["ffn_norm"][1]).astype(np.float32)
    sl = _slopes()
    pidx = np.arange(128, dtype=np.int64)
    for c in range(8):
        b, r = divmod(c, 4)
        hq = r
        slope = np.zeros((128, NPAIR), np.float32)
        idxB = np.zeros((128, 16), np.uint32)
        for i in range(NPAIR):
            slope[:, i] = sl[4 * hq + i]
            for rr in range(4):
                idxB[:, i * 4 + rr] = ((4 * hq + i) * 4 + rr) * 128 + pidx
        idxC = np.zeros((128, 128), np.uint32)
        for t in range(NTILE):
            for h in range(16):
                idxC[:, t * 16 + h] = ((h % 4) * 4 + r) * 4096 + (h // 4) * 1024 + t * 128 + pidx
        maps[c].update({"wC": wC, "ppC": ppC, "slope": slope, "idxB": idxB, "idxC": idxC})
    return maps


def kernel(**inputs):
    inp = {k: np.asarray(v) for k, v in inputs.items()}
    nc = _get("F", build_F)
    res = run_bass_kernel_spmd(nc, prep_F(inp), core_ids=list(range(8))).results
    out = np.empty((NB, S, D), np.float32)
    for c in range(8):
        b, r = divmod(c, 4)
        out[b, r * TOK:(r + 1) * TOK] = np.asarray(res[c]["xo"]).transpose(2, 1, 0).reshape(TOK, D)
    return out
```

```python
import numpy as np
from contextlib import ExitStack
import concourse.bass as bass
import concourse.mybir as mybir
from concourse.bass_utils import run_bass_kernel_spmd

F32 = mybir.dt.float32
BF16 = mybir.dt.bfloat16
AF = mybir.ActivationFunctionType
ALU = mybir.AluOpType
AX = mybir.AxisListType
U32 = mybir.dt.uint32

D = 2048
S = 16384
NB = 2
TOK = 4096
TT = 512
NTILE = TOK // TT
EPS = 1e-6
NEG = -1.0e30


_UID = [0]


def _uname(n):
    _UID[0] += 1
    return "%s_%d" % (n, _UID[0])


class Buf:
    __slots__ = ("name", "w", "r")

    def __init__(self, name):
        self.name = name
        self.w = None
        self.r = {}


class Prog:
    ENG = ("pe", "act", "dve", "pool", "sp")

    def __init__(self, nc, es):
        self.nc = nc
        self.es = es
        self.ops = {e: [] for e in self.ENG}
        self.sems = {}
        self.cnt = {}
        self.seen = {e: {} for e in self.ENG}
        for e in self.ENG[:4]:
            self._sem(e)

    def _sem(self, key):
        if key not in self.sems:
            self.sems[key] = self.es.enter_context(self.nc.semaphore("s_" + key))
            self.cnt[key] = 0
        return self.sems[key]

    def _deps(self, eng, reads, writes):
        need = {}
        for b in reads:
            if b.w is not None:
                s, v = b.w
                if v > need.get(s, 0):
                    need[s] = v
        for b in writes:
            if b.w is not None:
                s, v = b.w
                if v > need.get(s, 0):
                    need[s] = v
            for s, v in b.r.items():
                if v > need.get(s, 0):
                    need[s] = v
        waits = []
        for s, v in need.items():
            if s == "pe" and eng == "pe":
                continue
            if self.seen[eng].get(s, 0) < v:
                self.seen[eng][s] = v
                waits.append((s, v))
        return waits

    def _record(self, ev, reads, writes):
        s, v = ev
        for b in reads:
            if b.r.get(s, 0) < v:
                b.r[s] = v
        for b in writes:
            b.w = ev
            b.r = {}

    def op(self, eng, fn, reads=(), writes=(), inc=True):
        waits = self._deps(eng, reads, writes)
        if inc:
            self.cnt[eng] += 1
            ev = (eng, self.cnt[eng])
        else:
            ev = (eng, self.cnt[eng] + 1)
        self.ops[eng].append((waits, fn, (eng, 1) if inc else None))
        self._record(ev, reads, writes)

    def dma(self, q, semkey, fn, reads=(), writes=(), amt=16):
        waits = self._deps(q, reads, writes)
        self._sem(semkey)
        self.cnt[semkey] += amt
        ev = (semkey, self.cnt[semkey])
        self.ops[q].append((waits, fn, (semkey, amt)))
        self._record(ev, reads, writes)

    def wait_bufs(self, eng, bufs):
        waits = self._deps(eng, bufs, bufs)
        self.ops[eng].append((waits, None, None))

    def wait_all_dma(self, eng):
        waits = []
        for k, v in self.cnt.items():
            if k not in self.ENG and v > 0 and self.seen[eng].get(k, 0) < v:
                self.seen[eng][k] = v
                waits.append((k, v))
        self.ops[eng].append((waits, None, None))

    @staticmethod
    def merge(dst, srcs):
        for s_ in srcs:
            if s_.w is not None:
                s, v = s_.w
                if dst.r.get(s, 0) < v:
                    dst.r[s] = v
            for s, v in s_.r.items():
                if dst.r.get(s, 0) < v:
                    dst.r[s] = v

    def check(self):
        ptr = {e: 0 for e in self.ENG}
        val = {k: 0 for k in self.sems}
        prog = True
        while prog:
            prog = False
            for e in self.ENG:
                lst = self.ops[e]
                while ptr[e] < len(lst):
                    waits, fn, inc = lst[ptr[e]]
                    if any(val[s] < v for s, v in waits):
                        break
                    if inc is not None:
                        val[inc[0]] += inc[1]
                    ptr[e] += 1
                    prog = True
        for e in self.ENG:
            if ptr[e] < len(self.ops[e]):
                waits = self.ops[e][ptr[e]][0]
                raise RuntimeError("DEADLOCK: engine %s stuck at op %d waits=%s vals=%s" % (
                    e, ptr[e], waits, {s: val[s] for s, _ in waits}))

    def _emit(self, e, eng):
        for waits, fn, inc in self.ops[eng]:
            for s, v in waits:
                e.wait_ge(self.sems[s], v)
            if fn is not None:
                ins = fn(e)
                if inc is not None:
                    ins.then_inc(self.sems[inc[0]], inc[1])

    def barrier(self):
        for e in self.ENG:
            waits = []
            for k, v in self.cnt.items():
                if v > 0 and self.seen[e].get(k, 0) < v:
                    self.seen[e][k] = v
                    waits.append((k, v))
            self.ops[e].append((waits, None, None))

    def emit(self):
        if not hasattr(self, "all_ops"):
            self.all_ops = {e: [] for e in self.ENG}
        cur = self.ops
        for e in self.ENG:
            self.all_ops[e] += cur[e]
        self.ops = self.all_ops
        self.check()
        self.ops = cur
        self._emit_block()
        self.ops = {e: [] for e in self.ENG}

    def _emit_block(self):
        with self.nc.Block() as block:
            @block.tensor
            def _(e):
                self._emit(e, "pe")

            @block.scalar
            def _(e):
                self._emit(e, "act")

            @block.vector
            def _(e):
                self._emit(e, "dve")

            @block.gpsimd
            def _(e):
                self._emit(e, "pool")

            @block.sync
            def _(e):
                self._emit(e, "sp")


class TokBuilder:
    def __init__(self, nc, es, p, nblk, wsrc, wbf, tile_blocks, b_wbf=None):
        self.nc, self.es = nc, es
        self.p = p
        self.wsrc, self.wbf = wsrc, wbf
        self.nblk = nblk
        sb = lambda n, s, d: es.enter_context(nc.sbuf_tensor(_uname("sb_" + n), s, d))
        ps = lambda n, d=F32: es.enter_context(nc.psum_tensor(_uname(n), [128, 512], d))
        self.xt = sb("xt", [128, 16, TT], F32)
        self.b_x = [Buf("x%d" % i) for i in range(16)]
        self.ht = sb("ht", [128, 16, TT], BF16)
        self.b_h = Buf("h")
        self.a = sb("a", [128, 64, TT], BF16)
        self.b_a = [Buf("a%d" % i) for i in range(4)]
        self.cat = self.a[:, 0:16, :]
        self.b_cat = self.b_a[0]
        self.wslot = [sb("w%d" % i, [128, 8192], BF16) for i in range(3)]
        self.b_w = [Buf("w%d" % i) for i in range(3)]
        self.sq = [sb("sq%d" % i, [128, TT], BF16) for i in range(2)]
        self.b_sq = [Buf("sq%d" % i) for i in range(2)]
        self.rt = sb("rt", [128, TT], F32)
        self.b_rt = Buf("rt")
        self.rstd = sb("rstd", [128, TT], F32)
        self.b_rstd = Buf("rstd")
        self.ar = sb("arena", [128, 6208], F32)
        self.ones_bf = sb("ones_bf", [128, 128], BF16)
        self.epst = sb("epst", [128, 1], F32)
        self.b_const = Buf("const")
        self.mm = [ps("mm%d" % i) for i in range(4)]
        self.b_mm = [Buf("mm%d" % i) for i in range(4)]
        self.mmi = 0
        self.ps_n = ps("psn")
        self.b_psn = Buf("psn")
        self.aux = [ps("aux%d" % i) for i in range(3)]
        self.b_aux = [Buf("aux%d" % i) for i in range(3)]
        p.op("pool", lambda e: e.memset(self.ones_bf[:], 1.0), writes=[self.b_const])
        p.op("pool", lambda e: e.memset(self.epst[:], EPS), writes=[self.b_const])
        if b_wbf is None:
            self.b_wbf = [Buf("wbf%d" % i) for i in range(nblk)]
            for i in range(nblk):
                p.dma("pool", "cv%d" % i, lambda e, i=i: e.dma_start(out=wbf[i], in_=wsrc[i]), writes=[self.b_wbf[i]])
        else:
            self.b_wbf = b_wbf
        self.seq = []
        for t in range(NTILE):
            self.seq += tile_blocks
        self.g = 0
        self._wdma(0)
        self._wdma(1)

    def _wdma(self, g):
        if g >= len(self.seq):
            return
        blk = self.seq[g]
        s = g % 3
        self.p.dma("sp", "wl%d" % s, lambda e, s=s, blk=blk: e.dma_start(out=self.wslot[s][:], in_=self.wbf[blk]),
                   reads=[self.b_wbf[blk]], writes=[self.b_w[s]])

    def next_block(self, kc):
        g = self.g
        self._wdma(g + 2)
        self.g += 1
        s = g % 3
        return self.wslot[s][:].rearrange("p (k c) -> p k c", k=kc), self.b_w[s]

    def bank(self):
        i = self.mmi % 4
        self.mmi += 1
        return self.mm[i], self.b_mm[i]

    def mmul(self, out, lhsT, rhs, start, stop, reads, writes, inc):
        self.p.op("pe", lambda e: e.matmul(out, lhsT=lhsT, rhs=rhs, start=start, stop=stop), reads=reads, writes=writes, inc=inc)

    def act(self, out, in_, func, reads, writes, bias=None, scale=None):
        kw = {}
        if bias is not None:
            kw["bias"] = bias
        if scale is not None:
            kw["scale"] = scale
        self.p.op("act", lambda e: e.activation(out=out, in_=in_, func=func, **kw), reads=reads, writes=writes)

    def tt(self, eng, out, in0, in1, op, reads, writes):
        self.p.op(eng, lambda e: e.tensor_tensor(out=out, in0=in0, in1=in1, op=op), reads=reads, writes=writes)

    def stt(self, out, in0, scalar, in1, op0, op1, reads, writes):
        self.p.op("dve", lambda e: e.scalar_tensor_tensor(out=out, in0=in0, scalar=scalar, in1=in1, op0=op0, op1=op1), reads=reads, writes=writes)

    def ts(self, eng, out, in0, s1, s2, op0, op1, reads, writes):
        if op1 is None:
            self.p.op(eng, lambda e: e.tensor_scalar(out=out, in0=in0, scalar1=s1, scalar2=None, op0=op0), reads=reads, writes=writes)
        else:
            self.p.op(eng, lambda e: e.tensor_scalar(out=out, in0=in0, scalar1=s1, scalar2=s2, op0=op0, op1=op1), reads=reads, writes=writes)

    def copy(self, eng, out, in_, reads, writes):
        if eng == "act":
            self.p.op("act", lambda e: e.copy(out=out, in_=in_), reads=reads, writes=writes)
        else:
            self.p.op(eng, lambda e: e.tensor_copy(out=out, in_=in_), reads=reads, writes=writes)

    def rms_rstd(self, ps_ap, ps_buf, N, inv_n, rt, b_rt, rstd, b_rstd):
        self.act(rt[:, 0:N], ps_ap, AF.Sqrt, [ps_buf, self.b_const], [b_rt], bias=self.epst[:, 0:1], scale=inv_n)
        self.p.op("dve", lambda e: e.reciprocal(out=rstd[:, 0:N], in_=rt[:, 0:N]), reads=[b_rt], writes=[b_rstd])

    def norm(self, xk, xbufs, N, gain, gbuf, hk, hbuf):
        for kc in range(16):
            s, sbf = self.sq[kc % 2], self.b_sq[kc % 2]
            self.act(s[:, 0:N], xk(kc), AF.Square, [xbufs[kc]], [sbf])
            self.mmul(self.ps_n[:, 0:N], self.ones_bf[:], s[:, 0:N], kc == 0, kc == 15, [sbf, self.b_const], [self.b_psn], True)
        self.rms_rstd(self.ps_n[:, 0:N], self.b_psn, N, 1.0 / D, self.rt, self.b_rt, self.rstd, self.b_rstd)
        for kc in range(16):
            self.stt(hk(kc), xk(kc), gain[:, kc:kc + 1], self.rstd[:, 0:N], ALU.mult, ALU.mult,
                     [xbufs[kc], self.b_rstd, gbuf], [hbuf])

    def proj_resid(self, nblocks, src, b_src):
        for b4 in range(nblocks):
            W, bW = self.next_block(16)
            for pos in range(4):
                oc = b4 * 4 + pos
                ps_, bps = self.bank()
                for kc in range(16):
                    self.mmul(ps_[:], W[:, kc, pos * 128:(pos + 1) * 128], src[:, kc, :], kc == 0, kc == 15, [bW, b_src], [bps], kc == 15)
                self.tt("dve", self.xt[:, oc, :], ps_[:], self.xt[:, oc, :], ALU.add, [bps, self.b_x[oc]], [self.b_x[oc]])

    def mlp(self, rtmp, b_rtmp):
        for b16 in range(16):
            W, bW = self.next_block(16)
            for pos in range(4):
                hc = b16 * 4 + pos
                ps_, bps = self.bank()
                for kc in range(16):
                    self.mmul(ps_[:], W[:, kc, pos * 128:(pos + 1) * 128], self.ht[:, kc, :], kc == 0, kc == 15, [bW, self.b_h], [bps], kc == 15)
                r, br = rtmp[hc % 2], b_rtmp[hc % 2]
                self.act(r, ps_[:], AF.Relu, [bps], [br])
                self.tt("pool", self.a[:, hc, :], r, r, ALU.mult, [br], [self.b_a[hc // 16]])
        for oc in range(16):
            W, bW = self.next_block(64)
            ps_, bps = self.bank()
            for kc in range(64):
                self.mmul(ps_[:], W[:, kc, :], self.a[:, kc, :], kc == 0, kc == 63, [bW, self.b_a[kc // 16]], [bps], kc == 63)
            self.tt("dve", self.xt[:, oc, :], ps_[:], self.xt[:, oc, :], ALU.add, [bps, self.b_x[oc]], [self.b_x[oc]])


def _dram(nc, name, shape, dty, kind):
    return nc.dram_tensor(name, shape, dty, kind=kind).ap()


NBLK_A = 58
A_TILE_BLOCKS = list(range(58))
NBLK_C = 36
C_TILE_BLOCKS = list(range(36))


def phase_A(nc, p, T):
    xin, xh, wA, ppd, sgd, bsd, wsd = T["xT"], T["xh"], T["wA"], T["ppA"], T["sg"], T["bs"], T["wsT"]
    x1, qloc, kloc, vloc, kmloc, wbf = T["x1"], T["qloc"], T["kloc"], T["vloc"], T["kmloc"], T["wbfA"]
    qT = qloc.rearrange("(h d) t -> d h t", d=128)
    kT = kloc.rearrange("(h d) t -> d h t", d=128)
    vv4 = vloc.rearrange("(h p) (k d) -> p k h d", p=128, d=128)
    kmo = kmloc.rearrange("(h d) k -> d h k", d=128)
    es = ExitStack()
    with es:
        B = TokBuilder(nc, es, p, NBLK_A, wA, wbf, A_TILE_BLOCKS)
        sb = lambda n, s, d: es.enter_context(nc.sbuf_tensor(_uname("sb_" + n), s, d))
        pp = sb("pp", [128, 74], F32)
        sg = sb("sg", [128, 1024], F32)
        bshl = sb("bshl", [33, 1024], BF16)
        ones33 = sb("ones33", [33, 128], BF16)
        wsb = sb("wsb", [128, 8, 128], BF16)
        bsf = B.ar[0:33, 0:1024]
        bst = B.ar[0:33, 1024:2048]
        bshb = B.ar[0:33, 2048:2560].bitcast(BF16)
        wsf = B.ar[:, 3072:4096]
        wsm = B.ar[:, 4096:5120]
        xht = sb("xht", [128, 16, 2], F32)
        hht = sb("hht", [128, 16, 2], BF16)
        gch = sb("gch", [128, 8, 2], F32)
        carry = sb("carry", [128, 8, 2], F32)
        kmacc = sb("kmacc", [128, 16, 16], F32)
        ss4 = [sb("ss4%d" % i, [128, 4], F32) for i in range(2)]
        rt4 = [sb("rt4%d" % i, [128, 4], F32) for i in range(2)]
        rs4 = [sb("rs4%d" % i, [128, 4], F32) for i in range(2)]
        b_ss4 = [Buf("ss4"), Buf("ss4b")]
        b_rt4 = [Buf("rt4"), Buf("rt4b")]
        b_rs4 = [Buf("rs4"), Buf("rs4b")]
        b_pp, b_sg, b_bs, b_ws, b_xh, b_hh = Buf("pp"), Buf("sg"), Buf("bs"), Buf("ws"), Buf("xh"), Buf("hh")
        b_gch = [Buf("gch%d" % i) for i in range(8)]
        b_carry = [Buf("carry%d" % i) for i in range(8)]
        b_km = Buf("km")
        ar = B.ar
        TS = lambda i: ar[:, i * 512:(i + 1) * 512]
        gct = [TS(0), TS(1)]
        cvt = [TS(2), TS(3)]
        ugt = [TS(4), TS(5)]
        vtmp = [TS(6), TS(7)]
        sqv = TS(8)
        yt = [ar[:, 4608:4608 + 514], ar[:, 5632:5632 + 514]]
        b_T = [Buf("T%d" % i) for i in range(12)]
        b_gct, b_cvt, b_ugt, b_vtmp, b_sqv = b_T[0:2], b_T[2:4], b_T[4:6], b_T[6:8], b_T[8]
        b_yt = [[b_T[9], b_T[10]], [b_T[11]]]
        raw = [TS(0), TS(1)]
        b_raw = b_T[0:2]
        kn32 = [TS(2), TS(3)]
        b_kn32 = b_T[2:4]
        qkst = [ar[:, 2048:3072].bitcast(BF16).rearrange("p (h t) -> p h t", h=4),
                ar[:, 3072:4096].bitcast(BF16).rearrange("p (h t) -> p h t", h=4)]
        b_qkst = [[b_T[4], b_T[5]], [b_T[6], b_T[7]]]
        vst = [ar[:, 4096:5120].bitcast(BF16).rearrange("p (h t) -> p h t", h=4),
               ar[:, 5120:6144].bitcast(BF16).rearrange("p (h t) -> p h t", h=4)]
        b_vst = [[b_T[8], b_T[9]], [b_T[10], b_T[11]]]
        vn = B.a[:, 16:24, :].rearrange("p a b -> p (a b)").rearrange("p (t c) -> p t c", t=4)
        b_vn = B.b_a[1]

        p.dma("sp", "c0", lambda e: e.dma_start(out=pp[:], in_=ppd), writes=[b_pp])
        p.dma("sp", "c1", lambda e: e.dma_start(out=sg[:], in_=sgd), writes=[b_sg])
        p.dma("sp", "c2", lambda e: e.dma_start(out=bsf, in_=bsd), writes=[b_bs])
        p.dma("sp", "c3", lambda e: e.dma_start(out=wsf, in_=wsd), writes=[b_ws])
        p.dma("sp", "c4", lambda e: e.dma_start(out=xht[:], in_=xh), writes=[b_xh])
        p.op("pool", lambda e: e.memset(kmacc[:], 0.0), writes=[b_km])
        p.op("dve", lambda e: e.tensor_copy(out=bshb, in_=bsf), reads=[b_bs], writes=[b_bs])
        p.op("dve", lambda e: e.tensor_copy(out=bst, in_=bshb), reads=[b_bs], writes=[b_bs])
        p.op("dve", lambda e: e.tensor_tensor(out=bst, in0=bsf, in1=bst, op=ALU.subtract), reads=[b_bs], writes=[b_bs])
        p.op("dve", lambda e: e.tensor_copy(out=bshl[:], in_=bshb), reads=[b_bs], writes=[b_bs])
        p.op("dve", lambda e: e.tensor_copy(out=bshl[32:33, :], in_=bst[32:33, :]), reads=[b_bs], writes=[b_bs])
        p.op("pool", lambda e: e.memset(ones33[:], 1.0), writes=[b_bs])
        p.op("pool", lambda e: e.affine_select(out=wsm, in_=wsf, pattern=[[0, 8], [1, 128]], compare_op=ALU.is_ge,
                                                fill=0.0, base=0, channel_multiplier=-1), reads=[b_ws], writes=[b_ws])
        p.op("pool", lambda e: e.tensor_copy(out=wsb[:].rearrange("p g i -> p (g i)"), in_=wsm), reads=[b_ws], writes=[b_ws])
        for bt in b_T:
            Prog.merge(bt, [b_bs, b_ws])

        cw = lambda c, tap: pp[:, 48 + c * 3 + tap:48 + c * 3 + tap + 1]
        qg = pp[:, 72:73]
        kg = pp[:, 73:74]

        B.norm(lambda kc: xht[:, kc, :], [b_xh] * 16, 2, pp[:, 0:16], b_pp, lambda kc: hht[:, kc, :], b_hh)

        b_io = Buf("io")
        outs = [T["b_x1"], T["b_qloc"], T["b_kloc"], T["b_vloc"], T["b_kmloc"]]
        for t in range(NTILE):
            t0 = t * TT
            p.dma("sp", "xl", lambda e, t0=t0: e.dma_start(out=B.xt[:], in_=xin[:, :, t0:t0 + TT]), writes=B.b_x)
            B.norm(lambda kc: B.xt[:, kc, :], B.b_x, TT, pp[:, 0:16], b_pp, lambda kc: B.ht[:, kc, :], B.b_h)
            W = bW = None
            for i in range(24):
                blk, pos = divmod(i, 4)
                c, kind = divmod(i, 3)
                if pos == 0:
                    W, bW = B.next_block(16)
                ps_, bps = B.bank()
                for kc in range(16):
                    B.mmul(ps_[:], W[:, kc, pos * 128:(pos + 1) * 128], B.ht[:, kc, :], kc == 0, kc == 15, [bW, B.b_h], [bps], kc == 15)
                if t == 0 and kind < 2:
                    hp, bhp = B.aux[2], B.b_aux[2]
                    for kc in range(16):
                        B.mmul(hp[:, 0:2], W[:, kc, pos * 128:(pos + 1) * 128], hht[:, kc, :], kc == 0, kc == 15, [bW, b_hh], [bhp], kc == 15)
                    if kind == 0:
                        B.copy("act", gch[:, c, :], hp[:, 0:2], [bhp], [b_gch[c]])
                    else:
                        B.tt("dve", carry[:, c, :], hp[:, 0:2], gch[:, c, :], ALU.mult, [bhp, b_gch[c]], [b_carry[c]])
                if kind == 0:
                    B.copy("act", gct[c % 2], ps_[:], [bps], [b_gct[c % 2]])
                elif kind == 1:
                    y, by = yt[c % 2], b_yt[c % 2]
                    B.tt("dve", y[:, 2:514], ps_[:], gct[c % 2], ALU.mult, [bps, b_gct[c % 2]], by)
                    B.copy("pool", y[:, 0:2], carry[:, c, :], [b_carry[c]], by)
                    B.copy("pool", carry[:, c, :], y[:, 512:514], by, [b_carry[c]])
                    cv, bcv = cvt[c % 2], b_cvt[c % 2]
                    B.ts("dve", cv, y[:, 0:512], cw(c, 0), None, ALU.mult, None, by + [b_pp], [bcv])
                    B.stt(cv, y[:, 1:513], cw(c, 1), cv, ALU.mult, ALU.add, by + [b_pp, bcv], [bcv])
                    B.stt(cv, y[:, 2:514], cw(c, 2), cv, ALU.mult, ALU.add, by + [b_pp, bcv], [bcv])
                else:
                    B.tt("dve", B.cat[:, c, :], ps_[:], cvt[c % 2], ALU.mult, [bps, b_cvt[c % 2]], [B.b_cat])
            for vb in range(2):
                W, bW = B.next_block(16)
                for tc in range(4):
                    i2 = vb * 4 + tc
                    ps_, bps = B.bank()
                    for kc in range(16):
                        B.mmul(ps_[:], B.ht[:, kc, tc * 128:(tc + 1) * 128], W[:, kc, :], kc == 0, kc == 15, [bW, B.b_h], [bps], kc == 15)
                    vt, bvt = vtmp[i2 % 2], b_vtmp[i2 % 2]
                    B.act(vt, ps_[:], AF.Gelu_apprx_tanh, [bps], [bvt])
                    B.tt("pool", sqv, vt, vt, ALU.mult, [bvt], [b_sqv])
                    s4, r4, q4 = ss4[i2 % 2], rt4[i2 % 2], rs4[i2 % 2]
                    p.op("dve", lambda e, s4=s4: e.tensor_reduce(out=s4[:], in_=sqv.rearrange("p (g d) -> p g d", g=4), axis=AX.X, op=ALU.add),
                         reads=[b_sqv], writes=[b_ss4[i2 % 2]])
                    B.act(r4[:], s4[:], AF.Sqrt, [b_ss4[i2 % 2], B.b_const], [b_rt4[i2 % 2]], bias=B.epst[:, 0:1], scale=1.0 / 128)
                    p.op("dve", lambda e, r4=r4, q4=q4: e.reciprocal(out=q4[:], in_=r4[:]), reads=[b_rt4[i2 % 2]], writes=[b_rs4[i2 % 2]])
                    for g4 in range(4):
                        c0 = vb * 512 + g4 * 128
                        B.stt(vn[:, tc, c0:c0 + 128], vt[:, g4 * 128:(g4 + 1) * 128], q4[:, g4:g4 + 1], sg[:, c0:c0 + 128], ALU.mult, ALU.mult,
                              [bvt, b_rs4[i2 % 2], b_sg], [b_vn])
            for ub in range(2):
                W, bW = B.next_block(16)
                for pos in range(4):
                    c = ub * 4 + pos
                    ps_, bps = B.bank()
                    for kc in range(16):
                        B.mmul(ps_[:], W[:, kc, pos * 128:(pos + 1) * 128], B.ht[:, kc, :], kc == 0, kc == 15, [bW, B.b_h], [bps], kc == 15)
                    pm, bpm = B.aux[c % 2], B.b_aux[c % 2]
                    for tc in range(4):
                        B.mmul(pm[:, tc * 128:(tc + 1) * 128], vn[:, tc, c * 128:(c + 1) * 128], wsb[:, c, :], True, False, [b_vn, b_ws], [bpm], False)
                        B.mmul(pm[:, tc * 128:(tc + 1) * 128], ones33[:], bshl[:, c * 128:(c + 1) * 128], False, True, [b_bs], [bpm], tc == 3)
                    B.act(ugt[c % 2], ps_[:], AF.Gelu_apprx_tanh, [bps], [b_ugt[c % 2]])
                    B.tt("dve", B.cat[:, 8 + c, :], pm[:], ugt[c % 2], ALU.mult, [bpm, b_ugt[c % 2]], [B.b_cat])
            B.proj_resid(4, B.cat, B.b_cat)
            B.norm(lambda kc: B.xt[:, kc, :], B.b_x, TT, pp[:, 16:32], b_pp, lambda kc: B.ht[:, kc, :], B.b_h)
            B.mlp(gct, b_gct)
            p.dma("sp", "so_x", lambda e, t0=t0: e.dma_start(out=x1[:, :, t0:t0 + TT], in_=B.xt[:]), reads=B.b_x, writes=[outs[0]])
            B.norm(lambda kc: B.xt[:, kc, :], B.b_x, TT, pp[:, 32:48], b_pp, lambda kc: B.ht[:, kc, :], B.b_h)
            for qb in range(8):
                W, bW = B.next_block(16)
                isk = qb >= 4
                st, bst_ = qkst[qb % 2], b_qkst[qb % 2]
                for pos in range(4):
                    head = (qb % 4) * 4 + pos
                    ps_, bps = B.bank()
                    for kc in range(16):
                        B.mmul(ps_[:], W[:, kc, pos * 128:(pos + 1) * 128], B.ht[:, kc, :], kc == 0, kc == 15, [bW, B.b_h], [bps], kc == 15)
                    rw, brw = raw[pos % 2], b_raw[pos % 2]
                    B.copy("act", rw, ps_[:], [bps], [brw])
                    s, sbf = B.sq[pos % 2], B.b_sq[pos % 2]
                    B.tt("pool", s[:], rw, rw, ALU.mult, [brw], [sbf])
                    pn, bpn = B.aux[pos % 2], B.b_aux[pos % 2]
                    B.mmul(pn[:], B.ones_bf[:], s[:], True, True, [sbf, B.b_const], [bpn], True)
                    B.rms_rstd(pn[:], bpn, TT, 1.0 / 128, B.rt, B.b_rt, B.rstd, B.b_rstd)
                    kn, bkn = kn32[pos % 2], b_kn32[pos % 2]
                    B.stt(kn, rw, kg if isk else qg, B.rstd[:], ALU.mult, ALU.mult, [brw, B.b_rstd, b_pp], [bkn])
                    B.copy("pool", st[:, pos, :], kn, [bkn], bst_)
                    if isk:
                        p.op("dve", lambda e, kn=kn, head=head, t=t: e.tensor_reduce(
                            out=kmacc[:, head, 2 * t:2 * t + 2], in_=kn.rearrange("p (b k) -> p b k", b=2), axis=AX.X, op=ALU.add),
                            reads=[bkn], writes=[b_km])
                dst = kloc if isk else qloc
                h0 = (qb % 4) * 4
                row0 = (t * 2 + h0 // 8) * 1024 + (h0 % 8) * 128
                p.dma("sp", "so_q%d" % (qb % 2), lambda e, dst=dst, row0=row0, st=st: e.dma_start(
                    out=dst[row0:row0 + 512, :].rearrange("(hh d) c -> d hh c", d=128), in_=st),
                      reads=bst_, writes=[outs[2 if isk else 1]])
            for vb in range(4):
                W, bW = B.next_block(16)
                st, bst_ = vst[vb % 2], b_vst[vb % 2]
                for tc in range(4):
                    ps_, bps = B.bank()
                    for kc in range(16):
                        B.mmul(ps_[:], B.ht[:, kc, tc * 128:(tc + 1) * 128], W[:, kc, :], kc == 0, kc == 15, [bW, B.b_h], [bps], kc == 15)
                    B.copy("act", st[:, tc, :], ps_[:], [bps], bst_)
                for hh in range(4):
                    p.dma("sp", "so_v%d" % (vb % 2), lambda e, st=st, vb=vb, t=t, hh=hh: e.dma_start(
                        out=vloc[(t * 2 + (4 * vb + hh) // 8) * 1024 + ((4 * vb + hh) % 8) * 128:(t * 2 + (4 * vb + hh) // 8) * 1024 + ((4 * vb + hh) % 8) * 128 + 128, :].rearrange("p (k d) -> p k d", d=128),
                        in_=st[:, :, hh * 128:(hh + 1) * 128]),
                        reads=bst_, writes=[outs[3]])
            T["gatherA"](t)
            if t == 0 and T.get("convC") is not None:
                T["convC"]()
        p.op("act", lambda e: e.mul(out=kmacc[:], in_=kmacc[:], mul=1.0 / 256), reads=[b_km], writes=[b_km])
        p.dma("sp", "so_km", lambda e: e.dma_start(out=kmo, in_=kmacc[:]), reads=[b_km], writes=[outs[4]])
        T["gatherKM"]()
        p.barrier()
        p.emit()


def phase_C(nc, p, T):
    x1, og, wbf, ppd, xo, idxd = T["x1"], T["og"], T["wbfC"], T["ppC"], T["xo"], T["idxC"]
    es = ExitStack()
    with es:
        B = TokBuilder(nc, es, p, NBLK_C, None, wbf, C_TILE_BLOCKS, b_wbf=[T["b_wbfC"]] * NBLK_C)
        pp = es.enter_context(nc.sbuf_tensor("sb_ppc", [128, 16], F32))
        idx = es.enter_context(nc.sbuf_tensor("sb_idxc", [128, 128], U32))
        b_pp = Buf("pp")
        p.dma("sp", "c0", lambda e: e.dma_start(out=pp[:], in_=ppd), writes=[b_pp])
        p.dma("sp", "c1", lambda e: e.dma_start(out=idx[:], in_=idxd), writes=[b_pp])
        rtmp = [B.ar[:, 0:512], B.ar[:, 512:1024]]
        b_rtmp = [Buf("r0"), Buf("r1")]
        b_out = Buf("out")
        for t in range(NTILE):
            t0 = t * TT
            p.dma("sp", "xl", lambda e, t0=t0: e.dma_start(out=B.xt[:], in_=x1[:, :, t0:t0 + TT]), reads=[T["b_x1"]], writes=B.b_x)
            for h in range(16):
                p.dma("pool", "al%d" % (h % 2), lambda e, h=h, t=t: e.indirect_dma_start(
                    out=B.cat[:, h, :], out_offset=None, in_=og,
                    in_offset=bass.IndirectOffsetOnAxis(ap=idx[:, t * 16 + h:t * 16 + h + 1], axis=0)),
                    reads=[T["b_og"], b_pp], writes=[B.b_cat])
            B.proj_resid(4, B.cat, B.b_cat)
            B.norm(lambda kc: B.xt[:, kc, :], B.b_x, TT, pp[:, 0:16], b_pp, lambda kc: B.ht[:, kc, :], B.b_h)
            B.mlp(rtmp, b_rtmp)
            p.dma("sp", "so_x", lambda e, t0=t0: e.dma_start(out=xo[:, :, t0:t0 + TT], in_=B.xt[:]), reads=B.b_x, writes=[b_out])
        p.wait_all_dma("sp")
        p.barrier()
        p.emit()


NPAIR = 4
SCALE = 128 ** -0.5
BIGS = 30000.0 / SCALE


def phase_B(nc, p, T, npair=NPAIR, nqt=S // TT):
    qg, kg, vg, kmg, sld, oloc, idxd = T["qg"], T["kg"], T["vg"], T["kmg"], T["slope"], T["oloc"], T["idxB"]
    es = ExitStack()
    with es:
        sb = lambda n, s, d: es.enter_context(nc.sbuf_tensor(_uname("sb_" + n), s, d))
        psm = lambda n, d=F32: es.enter_context(nc.psum_tensor(_uname(n), [128, 512], d))
        qT = sb("qT", [128, S], BF16)
        kT = sb("kT", [128, S], BF16)
        vt = sb("vt", [128, 128, 128], BF16)
        kmf = sb("kmf", [128, 64], F32)
        kmb = sb("kmb", [128, 64], BF16)
        slope = sb("slope", [128, npair], F32)
        idx = sb("idxb", [128, 128], U32)
        idxk = sb("idxk", [128, 16], U32)
        ident = sb("ident", [128, 128], BF16)
        ones32 = sb("ones32", [128, 128], F32)
        E = sb("E", [65, 65, 128], BF16)
        iot = sb("iot", [128, 131], F32)
        kb = sb("kb", [128, npair, 131], F32)
        iot4 = sb("iot4", [128, 4], F32)
        qterm = sb("qterm", [128, npair, 4], F32)
        gm = [sb("gm%d" % i, [128, 64], F32) for i in range(2)]
        mx = [sb("mx%d" % i, [128, 8], F32) for i in range(2)]
        nm32 = [sb("nm32%d" % i, [128, 65], F32) for i in range(2)]
        nmb = [sb("nmb%d" % i, [128, 65], BF16) for i in range(8)]
        NMT = [sb("NMT%d" % i, [65, 512], BF16) for i in range(2)]
        pT = [sb("pT%d" % i, [128, 512], BF16) for i in range(3)]
        acc = sb("acc", [128, 512], F32)
        rl = sb("rl", [128, 512], F32)
        ot = [sb("ot%d" % i, [128, 512], BF16) for i in range(2)]
        ps_s = [psm("ps_s%d" % i) for i in range(3)]
        ps_o = [psm("ps_o%d" % i) for i in range(2)]
        ps_l = psm("ps_l")
        pg = psm("pg")
        pt = psm("pt", BF16)
        b_q, b_k, b_v, b_km, b_c = Buf("q"), Buf("k"), Buf("v"), Buf("km"), Buf("c")
        b_gm = [Buf("gm0"), Buf("gm1")]
        b_mx = [Buf("mx0"), Buf("mx1")]
        b_nm32 = [Buf("nm320"), Buf("nm321")]
        b_nmb = [Buf("nmb%d" % i) for i in range(8)]
        b_NMT = [Buf("NMT0"), Buf("NMT1")]
        b_pT = [Buf("pT%d" % i) for i in range(3)]
        b_acc, b_rl = Buf("acc"), Buf("rl")
        b_ot = [Buf("ot0"), Buf("ot1")]
        b_ps_s = [Buf("pss%d" % i) for i in range(3)]
        b_ps_o = [Buf("pso%d" % i) for i in range(2)]
        b_ps_l, b_pg, b_pt = Buf("psl"), Buf("pg"), Buf("pt")
        b_out = Buf("out")

        p.dma("sp", "c0", lambda e: e.dma_start(out=slope[:], in_=sld), writes=[b_c])
        p.dma("sp", "c1", lambda e: e.dma_start(out=idx[:], in_=idxd), writes=[b_c])
        p.dma("sp", "c2", lambda e: e.dma_start(out=idxk[:], in_=T["idxK"]), writes=[b_c])
        p.op("pool", lambda e: e.memset(ident[:], 1.0), writes=[b_c])
        p.op("pool", lambda e: e.affine_select(out=ident[:], in_=ident[:], pattern=[[1, 128]], compare_op=ALU.is_equal, fill=0.0,
                                                base=0, channel_multiplier=-1), reads=[b_c], writes=[b_c])
        p.op("pool", lambda e: e.memset(ones32[:], 1.0), writes=[b_c])
        p.op("pool", lambda e: e.memset(E[:], 1.0), writes=[b_c])
        p.op("pool", lambda e: e.affine_select(out=E[:], in_=E[:], pattern=[[1, 65], [0, 128]], compare_op=ALU.is_equal, fill=0.0,
                                                base=0, channel_multiplier=-1), reads=[b_c], writes=[b_c])
        p.op("pool", lambda e: e.iota(iot[:], pattern=[[128, 131]], base=-127 * 128, channel_multiplier=1,
                                       allow_small_or_imprecise_dtypes=True), writes=[b_c])
        p.op("pool", lambda e: e.iota(iot4[:], pattern=[[128, 4]], base=0, channel_multiplier=1,
                                       allow_small_or_imprecise_dtypes=True), writes=[b_c])
        for i in range(npair):
            p.op("dve", lambda e, i=i: e.tensor_scalar(out=kb[:, i, :], in0=iot[:], scalar1=slope[:, i:i + 1], scalar2=None, op0=ALU.mult),
                 reads=[b_c], writes=[b_c])
            p.op("dve", lambda e, i=i: e.tensor_scalar(out=qterm[:, i, :], in0=iot4[:], scalar1=slope[:, i:i + 1], scalar2=-1.0 / SCALE,
                                                        op0=ALU.mult, op1=ALU.mult), reads=[b_c], writes=[b_c])
        for i in range(2):
            p.op("pool", lambda e, i=i: e.memset(nm32[i][:], 0.0), writes=[b_nm32[i]])

        def gateA(pi, qt):
            par = qt % 2
            for j in range(4):
                own = 2 * qt + j // 2
                g2 = j % 2
                qs = qT[:, qt * TT + j * 128: qt * TT + (j + 1) * 128]
                p.op("pe", lambda e, qs=qs: e.matmul(pg[:, 0:64], lhsT=qs, rhs=kmb[:], start=True, stop=True), reads=[b_q, b_km], writes=[b_pg])
                if own > 0:
                    p.op("act", lambda e, g2=g2, own=own: e.copy(out=gm[g2][:, 0:own], in_=pg[:, 0:own]), reads=[b_pg], writes=[b_gm[g2]])
                p.op("dve", lambda e, g2=g2: e.max(out=mx[g2][:], in_=gm[g2][:]), reads=[b_gm[g2]], writes=[b_mx[g2]])
                p.op("dve", lambda e, g2=g2: e.tensor_scalar(out=nm32[g2][:, 0:64], in0=gm[g2][:], scalar1=mx[g2][:, 2:3], scalar2=-BIGS,
                                                             op0=ALU.is_lt, op1=ALU.mult), reads=[b_gm[g2], b_mx[g2]], writes=[b_nm32[g2]])
                nb_ = nmb[par * 4 + j]
                p.op("dve", lambda e, g2=g2, nb_=nb_, j=j: e.tensor_scalar(out=nb_[:], in0=nm32[g2][:], scalar1=qterm[:, pi, j:j + 1], scalar2=None,
                                                                          op0=ALU.add), reads=[b_nm32[g2], b_c], writes=[b_nmb[par * 4 + j]])

        def gateB(pi, qt):
            par = qt % 2
            for j in range(4):
                nb_ = nmb[par * 4 + j]
                p.op("pe", lambda e, nb_=nb_, j=j: e.transpose(out=pt[0:65, j * 128:(j + 1) * 128], in_=nb_[:], identity=ident[:]),
                     reads=[b_nmb[par * 4 + j], b_c], writes=[b_pt])
            p.op("act", lambda e: e.copy(out=NMT[par][:], in_=pt[0:65, :]), reads=[b_pt], writes=[b_NMT[par]])

        step = [0]

        def keyloop(pi, qt):
            par = qt % 2
            po, bpo = ps_o[par], b_ps_o[par]
            nkt = 4 * qt + 4
            pend = []
            for kt in range(nkt):
                n = kt // 2
                iin = kt - 4 * qt
                if iin < 0:
                    ranges = [(0, 512, n)]
                elif iin == 0:
                    ranges = [(0, 256, 64), (256, 512, n)]
                elif iin == 1:
                    ranges = [(128, 256, 64), (256, 512, n)]
                elif iin == 2:
                    ranges = [(256, 512, 64)]
                else:
                    ranges = [(384, 512, 64)]
                c0 = ranges[0][0]
                si = step[0] % 3
                step[0] += 1
                pss, bpss = ps_s[si], b_ps_s[si]
                pTt, bpT = pT[si], b_pT[si]
                q0 = qt * TT
                p.op("pe", lambda e, pss=pss, kt=kt, c0=c0, q0=q0: e.matmul(pss[:, c0:512], lhsT=kT[:, kt * 128:(kt + 1) * 128],
                                                                         rhs=qT[:, q0 + c0:q0 + 512], start=True, stop=False),
                     reads=[b_k, b_q], writes=[bpss], inc=False)
                for ri, (a_, b_, row) in enumerate(ranges):
                    last = ri == len(ranges) - 1
                    p.op("pe", lambda e, pss=pss, a_=a_, b_=b_, row=row, last=last: e.matmul(
                        pss[:, a_:b_], lhsT=E[:, row, :], rhs=NMT[par][:, a_:b_], start=False, stop=last),
                        reads=[b_c, b_NMT[par]], writes=[bpss], inc=last)
                while pend:
                    pend.pop(0)()
                mi = kt - 4 * qt + 127
                p.op("act", lambda e, pss=pss, pTt=pTt, c0=c0, mi=mi: e.activation(out=pTt[:, c0:512], in_=pss[:, c0:512], func=AF.Exp,
                                                                                 bias=kb[:, pi, mi:mi + 1], scale=SCALE),
                     reads=[bpss, b_c], writes=[bpT])
                if iin >= 0:
                    j = iin
                    p.op("pool", lambda e, pTt=pTt, j=j: e.affine_select(out=pTt[:, j * 128:(j + 1) * 128], in_=pTt[:, j * 128:(j + 1) * 128],
                                                                       pattern=[[1, 128]], compare_op=ALU.is_ge, fill=0.0, base=0,
                                                                       channel_multiplier=-1), reads=[bpT], writes=[bpT])
                if kt == 0:
                    p.op("dve", lambda e, pTt=pTt: e.tensor_copy(out=acc[:], in_=pTt[:]), reads=[bpT], writes=[b_acc])
                else:
                    p.op("dve", lambda e, pTt=pTt, c0=c0: e.tensor_tensor(out=acc[:, c0:512], in0=acc[:, c0:512], in1=pTt[:, c0:512], op=ALU.add),
                         reads=[bpT, b_acc], writes=[b_acc])
                pend.append(lambda po=po, kt=kt, c0=c0, pTt=pTt, nkt=nkt, bpT=bpT: p.op(
                    "pe", lambda e: e.matmul(po[:, c0:512], lhsT=vt[:, kt, :], rhs=pTt[:, c0:512], start=(kt == 0), stop=(kt == nkt - 1)),
                    reads=[b_v, bpT], writes=[bpo], inc=True))
            while pend:
                pend.pop(0)()
            p.op("pe", lambda e: e.matmul(ps_l[:], lhsT=ones32[:], rhs=acc[:], start=True, stop=True), reads=[b_acc, b_c], writes=[b_ps_l])
            p.op("dve", lambda e: e.reciprocal(out=rl[:], in_=ps_l[:]), reads=[b_ps_l], writes=[b_rl])
            o_, bo = ot[par], b_ot[par]
            p.op("dve", lambda e, o_=o_, po=po: e.tensor_tensor(out=o_[:], in0=po[:], in1=rl[:], op=ALU.mult), reads=[bpo, b_rl], writes=[bo])
            row0 = (pi * 32 + qt) * 128
            p.dma("sp", "so%d" % par, lambda e, o_=o_, row0=row0: e.dma_start(out=oloc[row0:row0 + 128, :], in_=o_[:]), reads=[bo], writes=[T["b_oloc"]])
            if qt % 8 == 7:
                T["gatherB"](pi * 4 + qt // 8)

        for pi in range(npair):
            vflat = vt[:].rearrange("p t d -> p (t d)")
            first = True
            for r in range(4):
                iok = bass.IndirectOffsetOnAxis(ap=idxk[:, pi * 4 + r:pi * 4 + r + 1], axis=0)
                p.dma("pool", "lm", lambda e, r=r, iok=iok: e.indirect_dma_start(out=kmf[:, r * 16:(r + 1) * 16], out_offset=None, in_=kmg, in_offset=iok),
                      reads=[T["b_kmg"], b_c], writes=[b_km] if first else [])
                for t in range(NTILE):
                    col = (pi * 4 + r) * 8 + t
                    io = bass.IndirectOffsetOnAxis(ap=idx[:, col:col + 1], axis=0)
                    c0 = r * TOK + t * TT
                    p.dma("pool", "lq", lambda e, c0=c0, io=io: e.indirect_dma_start(out=qT[:, c0:c0 + TT], out_offset=None, in_=qg, in_offset=io),
                          reads=[T["b_qg"], b_c], writes=[b_q] if first else [])
                    p.dma("pool", "lk", lambda e, c0=c0, io=io: e.indirect_dma_start(out=kT[:, c0:c0 + TT], out_offset=None, in_=kg, in_offset=io),
                          reads=[T["b_kg"], b_c], writes=[b_k] if first else [])
                    p.dma("pool", "lv", lambda e, c0=c0, io=io: e.indirect_dma_start(out=vflat[:, c0:c0 + TT], out_offset=None, in_=vg, in_offset=io),
                          reads=[T["b_vg"], b_c], writes=[b_v] if first else [])
                    first = False
            b_q.w, b_k.w, b_v.w, b_km.w = ("lq", p.cnt["lq"]), ("lk", p.cnt["lk"]), ("lv", p.cnt["lv"]), ("lm", p.cnt["lm"])
            p.op("dve", lambda e: e.tensor_copy(out=kmb[:], in_=kmf[:]), reads=[b_km], writes=[b_km])
            for i in range(2):
                p.op("pool", lambda e, i=i: e.memset(gm[i][:], NEG), writes=[b_gm[i]])
            gateA(pi, 0)
            gateB(pi, 0)
            for qt in range(nqt):
                if qt + 1 < nqt:
                    gateA(pi, qt + 1)
                keyloop(pi, qt)
                if qt + 1 < nqt:
                    gateB(pi, qt + 1)
        p.barrier()
        p.emit()


def _tile16(W, n0, ncols=512):
    return np.ascontiguousarray(W[:, n0:n0 + ncols].reshape(16, 128, ncols).transpose(1, 0, 2)).reshape(128, 16 * ncols)


def _tile64(W, n0):
    return np.ascontiguousarray(W[:, n0:n0 + 128].reshape(64, 128, 128).transpose(1, 0, 2)).reshape(128, 8192)


def _fm(v):
    return np.ascontiguousarray(v.reshape(16, 128).T)


def _slopes():
    return (2.0 ** (-8.0 * np.arange(1, 17, dtype=np.float32) / 16)).astype(np.float32)


_CACHE = {}


def _get(name, fn):
    if name not in _CACHE:
        _CACHE[name] = fn()
    return _CACHE[name]


def prep_A(inp):
    x = inp["x"]
    w_in = inp["w_in"][0]
    order = []
    for c in range(8):
        order += list(range(1024 + 128 * c, 1024 + 128 * (c + 1)))
        order += list(range(2048 + 128 * c, 2048 + 128 * (c + 1)))
        order += list(range(0 + 128 * c, 0 + 128 * (c + 1)))
    order += list(range(4096, 5120))
    order += list(range(3072, 4096))
    w_in_p = w_in[:, order]
    blocks = [_tile16(w_in_p, n0) for n0 in range(0, 5120, 512)]
    blocks += [_tile16(inp["w_mix_out"][0], n0) for n0 in range(0, 2048, 512)]
    blocks += [_tile16(inp["w_up"][0], n0) for n0 in range(0, 8192, 512)]
    blocks += [_tile64(inp["w_down"][0], n0) for n0 in range(0, 2048, 128)]
    blocks += [_tile16(inp["w_qkv"][0], n0) for n0 in range(0, 6144, 512)]
    wA = np.stack(blocks).astype(np.float32)
    assert wA.shape == (NBLK_A, 128, 8192)
    pp = np.zeros((128, 74), np.float32)
    pp[:, 0:16] = _fm(inp["mix_norm"][0])
    pp[:, 16:32] = _fm(inp["ffn_norm"][0])
    pp[:, 32:48] = _fm(inp["mix_norm"][1])
    pp[:, 48:72] = inp["conv_w"][0].reshape(8, 128, 3).transpose(1, 0, 2).reshape(128, 24)
    pp[:, 72] = inp["q_gain"][0]
    pp[:, 73] = inp["k_gain"][0]
    sg = np.ascontiguousarray(np.broadcast_to(inp["sgu_gain"][0][None, :], (128, 1024))).astype(np.float32)
    bs = np.zeros((33, 1024), np.float32)
    bs[0] = inp["b_s"][0].reshape(1024)
    bs[32] = inp["b_s"][0].reshape(1024)
    wsT = np.ascontiguousarray(inp["w_s"][0].transpose(2, 0, 1)).reshape(128, 1024).astype(np.float32)
    maps = []
    for c in range(8):
        b, r = divmod(c, 4)
        s0 = r * TOK
        xs = x[b, s0:s0 + TOK]
        xT = np.ascontiguousarray(xs.reshape(TOK, 16, 128).transpose(2, 1, 0))
        xh = np.zeros((128, 16, 2), np.float32)
        if r > 0:
            xh[:] = x[b, s0 - 2:s0].reshape(2, 16, 128).transpose(2, 1, 0)
        maps.append({"xT": xT, "xh": xh, "wA": wA, "pp": pp, "sg": sg, "bs": bs, "wsT": wsT})
    return maps


def build_F():
    nc = bass.Bass("TRN2", target_bir_lowering=False)
    T = {}
    ext = lambda n, sh, dt: _dram(nc, n, sh, dt, "ExternalInput")
    itn = lambda n, sh, dt: _dram(nc, n, sh, dt, "Internal")
    T["xT"] = ext("xT", [128, 16, TOK], F32)
    T["xh"] = ext("xh", [128, 16, 2], F32)
    T["wA"] = ext("wA", [NBLK_A, 128, 8192], F32)
    T["wC"] = ext("wC", [NBLK_C, 128, 8192], F32)
    T["ppA"] = ext("pp", [128, 74], F32)
    T["ppC"] = ext("ppC", [128, 16], F32)
    T["sg"] = ext("sg", [128, 1024], F32)
    T["bs"] = ext("bs", [33, 1024], F32)
    T["wsT"] = ext("wsT", [128, 1024], F32)
    T["slope"] = ext("slope", [128, NPAIR], F32)
    T["idxB"] = ext("idxB", [128, 128], U32)
    T["idxK"] = ext("idxK", [128, 16], U32)
    T["idxC"] = ext("idxC", [128, 128], U32)
    T["xo"] = _dram(nc, "xo", [128, 16, TOK], F32, "ExternalOutput")
    T["wbfA"] = itn("wbfA", [NBLK_A, 128, 8192], BF16)
    T["wbfC"] = itn("wbfC", [NBLK_C, 128, 8192], BF16)
    T["x1"] = itn("x1", [128, 16, TOK], F32)
    for nm in ("q", "k", "v"):
        T[nm + "loc"] = itn(nm + "loc", [16 * 1024, TT], BF16)
        T[nm + "g"] = itn(nm + "g", [16 * 4096, TT], BF16)
    T["kmloc"] = itn("kmloc", [2048, 16], F32)
    T["kmg"] = itn("kmg", [4 * 2048, 16], F32)
    T["oloc"] = itn("oloc", [NPAIR * 32 * 128, TT], BF16)
    T["og"] = itn("og", [4 * NPAIR * 32 * 128, TT], BF16)
    for nm in ("x1", "qloc", "kloc", "vloc", "kmloc", "qg", "kg", "vg", "kmg", "oloc", "og", "wbfC"):
        T["b_" + nm] = Buf(nm)
    groups = [[0, 1, 2, 3], [4, 5, 6, 7]]
    es = ExitStack()
    with es:
        p = Prog(nc, es)

        def convC():
            for i in range(NBLK_C):
                p.dma("pool", "cvC", lambda e, i=i: e.dma_start(out=T["wbfC"][i], in_=T["wC"][i]))
            T["b_wbfC"].w = ("cvC", p.cnt["cvC"])

        def gatherA(t):
            for nm in ("q", "k", "v"):
                for hh in range(2):
                    c = t * 2 + hh
                    p.dma("pool", "cc" + nm, lambda e, nm=nm, c=c: e.collective_compute(
                        "AllGather", ALU.bypass, replica_groups=groups,
                        ins=[T[nm + "loc"][c * 1024:(c + 1) * 1024, :].opt()], outs=[T[nm + "g"][c * 4096:(c + 1) * 4096, :].opt()]),
                        reads=[T["b_" + nm + "loc"]], writes=[], amt=1)
                T["b_" + nm + "g"].w = ("cc" + nm, p.cnt["cc" + nm])

        def gatherKM():
            p.dma("pool", "cckm", lambda e: e.collective_compute(
                "AllGather", ALU.bypass, replica_groups=groups, ins=[T["kmloc"].opt()], outs=[T["kmg"].opt()]),
                reads=[T["b_kmloc"]], writes=[T["b_kmg"]], amt=1)

        def gatherB(c):
            p.dma("pool", "cco", lambda e, c=c: e.collective_compute(
                "AllGather", ALU.bypass, replica_groups=groups,
                ins=[T["oloc"][c * 1024:(c + 1) * 1024, :].opt()], outs=[T["og"][c * 4096:(c + 1) * 4096, :].opt()]),
                reads=[T["b_oloc"]], writes=[], amt=1)
            T["b_og"].w = ("cco", p.cnt["cco"])

        T["gatherKM"] = gatherKM
        T["convC"], T["gatherA"], T["gatherB"] = convC, gatherA, gatherB
        phase_A(nc, p, T)
        phase_B(nc, p, T)
        phase_C(nc, p, T)
    return nc


def prep_F(inp):
    maps = prep_A(inp)
    blocks = [_tile16(inp["w_attn_out"][0], n0) for n0 in range(0, 2048, 512)]
    blocks += [_tile16(inp["w_up"][1], n0) for n0 in range(0, 8192, 512)]
    blocks += [_tile64(inp["w_down"][1], n0) for n0 in range(0, 2048, 128)]
    wC = np.stack(blocks).astype(np.float32)
    ppC = _fm(inp["ffn_norm"][1]).astype(np.float32)
    sl = _slopes()
    pidx = np.arange(128, dtype=np.int64)
    for c in range(8):
        b, r = divmod(c, 4)
        hq = r
        slope = np.zeros((128, NPAIR), np.float32)
        idxB = np.zeros((128, 128), np.uint32)
        idxK = np.zeros((128, 16), np.uint32)
        for i in range(NPAIR):
            h = 4 * hq + i
            slope[:, i] = sl[h]
            for rr in range(4):
                idxK[:, i * 4 + rr] = rr * 2048 + h * 128 + pidx
                for t in range(NTILE):
                    idxB[:, (i * 4 + rr) * 8 + t] = ((t * 2 + h // 8) * 4 + rr) * 1024 + (h % 8) * 128 + pidx
        idxC = np.zeros((128, 128), np.uint32)
        for t in range(NTILE):
            for h in range(16):
                idxC[:, t * 16 + h] = ((h % 4) * 4 + r) * 4096 + (h // 4) * 1024 + t * 128 + pidx
        maps[c].update({"wC": wC, "ppC": ppC, "slope": slope, "idxB": idxB, "idxK": idxK, "idxC": idxC})
    return maps


def kernel(**inputs):
    inp = {k: np.asarray(v) for k, v in inputs.items()}
    nc = _get("F", build_F)
    res = run_bass_kernel_spmd(nc, prep_F(inp), core_ids=list(range(8))).results
    out = np.empty((NB, S, D), np.float32)
    for c in range(8):
        b, r = divmod(c, 4)
        out[b, r * TOK:(r + 1) * TOK] = np.asarray(res[c]["xo"]).transpose(2, 1, 0).reshape(TOK, D)
    return out
```

```python
import numpy as np
from contextlib import ExitStack
import concourse.bass as bass
import concourse.mybir as mybir
from concourse.bass_utils import run_bass_kernel_spmd

F32 = mybir.dt.float32
BF16 = mybir.dt.bfloat16
AF = mybir.ActivationFunctionType
ALU = mybir.AluOpType
AX = mybir.AxisListType
U32 = mybir.dt.uint32

D = 2048
S = 16384
NB = 2
TOK = 4096
TT = 512
NTILE = TOK // TT
EPS = 1e-6
NEG = -1.0e30


_UID = [0]


def _uname(n):
    _UID[0] += 1
    return "%s_%d" % (n, _UID[0])


class Buf:
    __slots__ = ("name", "w", "r")

    def __init__(self, name):
        self.name = name
        self.w = None
        self.r = {}


class Prog:
    ENG = ("pe", "act", "dve", "pool", "sp")

    def __init__(self, nc, es):
        self.nc = nc
        self.es = es
        self.ops = {e: [] for e in self.ENG}
        self.sems = {}
        self.cnt = {}
        self.seen = {e: {} for e in self.ENG}
        for e in self.ENG[:4]:
            self._sem(e)

    def _sem(self, key):
        if key not in self.sems:
            self.sems[key] = self.es.enter_context(self.nc.semaphore("s_" + key))
            self.cnt[key] = 0
        return self.sems[key]

    def _deps(self, eng, reads, writes):
        need = {}
        for b in reads:
            if b.w is not None:
                s, v = b.w
                if v > need.get(s, 0):
                    need[s] = v
        for b in writes:
            if b.w is not None:
                s, v = b.w
                if v > need.get(s, 0):
                    need[s] = v
            for s, v in b.r.items():
                if v > need.get(s, 0):
                    need[s] = v
        waits = []
        for s, v in need.items():
            if s == "pe" and eng == "pe":
                continue
            if self.seen[eng].get(s, 0) < v:
                self.seen[eng][s] = v
                waits.append((s, v))
        return waits

    def _record(self, ev, reads, writes):
        s, v = ev
        for b in reads:
            if b.r.get(s, 0) < v:
                b.r[s] = v
        for b in writes:
            b.w = ev
            b.r = {}

    def op(self, eng, fn, reads=(), writes=(), inc=True):
        waits = self._deps(eng, reads, writes)
        if inc:
            self.cnt[eng] += 1
            ev = (eng, self.cnt[eng])
        else:
            ev = (eng, self.cnt[eng] + 1)
        self.ops[eng].append((waits, fn, (eng, 1) if inc else None))
        self._record(ev, reads, writes)

    def dma(self, q, semkey, fn, reads=(), writes=(), amt=16):
        waits = self._deps(q, reads, writes)
        self._sem(semkey)
        self.cnt[semkey] += amt
        ev = (semkey, self.cnt[semkey])
        self.ops[q].append((waits, fn, (semkey, amt)))
        self._record(ev, reads, writes)

    def wait_bufs(self, eng, bufs):
        waits = self._deps(eng, bufs, bufs)
        self.ops[eng].append((waits, None, None))

    def wait_all_dma(self, eng):
        waits = []
        for k, v in self.cnt.items():
            if k not in self.ENG and v > 0 and self.seen[eng].get(k, 0) < v:
                self.seen[eng][k] = v
                waits.append((k, v))
        self.ops[eng].append((waits, None, None))

    @staticmethod
    def merge(dst, srcs):
        for s_ in srcs:
            if s_.w is not None:
                s, v = s_.w
                if dst.r.get(s, 0) < v:
                    dst.r[s] = v
            for s, v in s_.r.items():
                if dst.r.get(s, 0) < v:
                    dst.r[s] = v

    def check(self):
        ptr = {e: 0 for e in self.ENG}
        val = {k: 0 for k in self.sems}
        prog = True
        while prog:
            prog = False
            for e in self.ENG:
                lst = self.ops[e]
                while ptr[e] < len(lst):
                    waits, fn, inc = lst[ptr[e]]
                    if any(val[s] < v for s, v in waits):
                        break
                    if inc is not None:
                        val[inc[0]] += inc[1]
                    ptr[e] += 1
                    prog = True
        for e in self.ENG:
            if ptr[e] < len(self.ops[e]):
                waits = self.ops[e][ptr[e]][0]
                raise RuntimeError("DEADLOCK: engine %s stuck at op %d waits=%s vals=%s" % (
                    e, ptr[e], waits, {s: val[s] for s, _ in waits}))

    def _emit(self, e, eng):
        for waits, fn, inc in self.ops[eng]:
            for s, v in waits:
                e.wait_ge(self.sems[s], v)
            if fn is not None:
                ins = fn(e)
                if inc is not None:
                    ins.then_inc(self.sems[inc[0]], inc[1])

    def barrier(self):
        for e in self.ENG:
            waits = []
            for k, v in self.cnt.items():
                if v > 0 and self.seen[e].get(k, 0) < v:
                    self.seen[e][k] = v
                    waits.append((k, v))
            self.ops[e].append((waits, None, None))

    def emit(self):
        if not hasattr(self, "all_ops"):
            self.all_ops = {e: [] for e in self.ENG}
        cur = self.ops
        for e in self.ENG:
            self.all_ops[e] += cur[e]
        self.ops = self.all_ops
        self.check()
        self.ops = cur
        self._emit_block()
        self.ops = {e: [] for e in self.ENG}

    def _emit_block(self):
        with self.nc.Block() as block:
            @block.tensor
            def _(e):
                self._emit(e, "pe")

            @block.scalar
            def _(e):
                self._emit(e, "act")

            @block.vector
            def _(e):
                self._emit(e, "dve")

            @block.gpsimd
            def _(e):
                self._emit(e, "pool")

            @block.sync
            def _(e):
                self._emit(e, "sp")


class TokBuilder:
    def __init__(self, nc, es, p, nblk, wsrc, wbf, tile_blocks, b_wbf=None):
        self.nc, self.es = nc, es
        self.p = p
        self.wsrc, self.wbf = wsrc, wbf
        self.nblk = nblk
        sb = lambda n, s, d: es.enter_context(nc.sbuf_tensor(_uname("sb_" + n), s, d))
        ps = lambda n, d=F32: es.enter_context(nc.psum_tensor(_uname(n), [128, 512], d))
        self.xt = sb("xt", [128, 16, TT], F32)
        self.b_x = [Buf("x%d" % i) for i in range(16)]
        self.ht = sb("ht", [128, 16, TT], BF16)
        self.b_h = Buf("h")
        self.a = sb("a", [128, 64, TT], BF16)
        self.b_a = [Buf("a%d" % i) for i in range(4)]
        self.cat = self.a[:, 0:16, :]
        self.b_cat = self.b_a[0]
        self.wslot = [sb("w%d" % i, [128, 8192], BF16) for i in range(3)]
        self.b_w = [Buf("w%d" % i) for i in range(3)]
        self.sq = [sb("sq%d" % i, [128, TT], BF16) for i in range(2)]
        self.b_sq = [Buf("sq%d" % i) for i in range(2)]
        self.rt = sb("rt", [128, TT], F32)
        self.b_rt = Buf("rt")
        self.rstd = sb("rstd", [128, TT], F32)
        self.b_rstd = Buf("rstd")
        self.ar = sb("arena", [128, 6208], F32)
        self.ones_bf = sb("ones_bf", [128, 128], BF16)
        self.epst = sb("epst", [128, 1], F32)
        self.b_const = Buf("const")
        self.mm = [ps("mm%d" % i) for i in range(4)]
        self.b_mm = [Buf("mm%d" % i) for i in range(4)]
        self.mmi = 0
        self.ps_n = ps("psn")
        self.b_psn = Buf("psn")
        self.aux = [ps("aux%d" % i) for i in range(3)]
        self.b_aux = [Buf("aux%d" % i) for i in range(3)]
        p.op("pool", lambda e: e.memset(self.ones_bf[:], 1.0), writes=[self.b_const])
        p.op("pool", lambda e: e.memset(self.epst[:], EPS), writes=[self.b_const])
        if b_wbf is None:
            self.b_wbf = [Buf("wbf%d" % i) for i in range(nblk)]
            self.conv_next = 0
            self.conv_ahead(12)
        else:
            self.b_wbf = b_wbf
        self.seq = []
        for t in range(NTILE):
            self.seq += tile_blocks
        self.g = 0
        self._wdma(0)
        self._wdma(1)

    def conv_ahead(self, upto):
        while getattr(self, "conv_next", None) is not None and self.conv_next < min(upto, self.nblk):
            i = self.conv_next
            self.p.dma("pool", "cv%d" % i, lambda e, i=i: e.dma_start(out=self.wbf[i], in_=self.wsrc[i]), writes=[self.b_wbf[i]])
            self.conv_next += 1

    def _wdma(self, g):
        if g < self.nblk:
            self.conv_ahead(g + 8)
        if g >= len(self.seq):
            return
        blk = self.seq[g]
        s = g % 3
        self.p.dma("sp", "wl%d" % s, lambda e, s=s, blk=blk: e.dma_start(out=self.wslot[s][:], in_=self.wbf[blk]),
                   reads=[self.b_wbf[blk]], writes=[self.b_w[s]])

    def next_block(self, kc):
        g = self.g
        self._wdma(g + 2)
        self.g += 1
        s = g % 3
        return self.wslot[s][:].rearrange("p (k c) -> p k c", k=kc), self.b_w[s]

    def bank(self):
        i = self.mmi % 4
        self.mmi += 1
        return self.mm[i], self.b_mm[i]

    def mmul(self, out, lhsT, rhs, start, stop, reads, writes, inc):
        self.p.op("pe", lambda e: e.matmul(out, lhsT=lhsT, rhs=rhs, start=start, stop=stop), reads=reads, writes=writes, inc=inc)

    def act(self, out, in_, func, reads, writes, bias=None, scale=None):
        kw = {}
        if bias is not None:
            kw["bias"] = bias
        if scale is not None:
            kw["scale"] = scale
        self.p.op("act", lambda e: e.activation(out=out, in_=in_, func=func, **kw), reads=reads, writes=writes)

    def tt(self, eng, out, in0, in1, op, reads, writes):
        self.p.op(eng, lambda e: e.tensor_tensor(out=out, in0=in0, in1=in1, op=op), reads=reads, writes=writes)

    def stt(self, out, in0, scalar, in1, op0, op1, reads, writes):
        self.p.op("dve", lambda e: e.scalar_tensor_tensor(out=out, in0=in0, scalar=scalar, in1=in1, op0=op0, op1=op1), reads=reads, writes=writes)

    def ts(self, eng, out, in0, s1, s2, op0, op1, reads, writes):
        if op1 is None:
            self.p.op(eng, lambda e: e.tensor_scalar(out=out, in0=in0, scalar1=s1, scalar2=None, op0=op0), reads=reads, writes=writes)
        else:
            self.p.op(eng, lambda e: e.tensor_scalar(out=out, in0=in0, scalar1=s1, scalar2=s2, op0=op0, op1=op1), reads=reads, writes=writes)

    def copy(self, eng, out, in_, reads, writes):
        if eng == "act":
            self.p.op("act", lambda e: e.copy(out=out, in_=in_), reads=reads, writes=writes)
        else:
            self.p.op(eng, lambda e: e.tensor_copy(out=out, in_=in_), reads=reads, writes=writes)

    def rms_rstd(self, ps_ap, ps_buf, N, inv_n, rt, b_rt, rstd, b_rstd):
        self.act(rt[:, 0:N], ps_ap, AF.Sqrt, [ps_buf, self.b_const], [b_rt], bias=self.epst[:, 0:1], scale=inv_n)
        self.p.op("dve", lambda e: e.reciprocal(out=rstd[:, 0:N], in_=rt[:, 0:N]), reads=[b_rt], writes=[b_rstd])

    def norm(self, xk, xbufs, N, gain, gbuf, hk, hbuf):
        for kc in range(16):
            s, sbf = self.sq[kc % 2], self.b_sq[kc % 2]
            self.act(s[:, 0:N], xk(kc), AF.Square, [xbufs[kc]], [sbf])
            self.mmul(self.ps_n[:, 0:N], self.ones_bf[:], s[:, 0:N], kc == 0, kc == 15, [sbf, self.b_const], [self.b_psn], True)
        self.rms_rstd(self.ps_n[:, 0:N], self.b_psn, N, 1.0 / D, self.rt, self.b_rt, self.rstd, self.b_rstd)
        for kc in range(16):
            self.stt(hk(kc), xk(kc), gain[:, kc:kc + 1], self.rstd[:, 0:N], ALU.mult, ALU.mult,
                     [xbufs[kc], self.b_rstd, gbuf], [hbuf])

    def proj_resid(self, nblocks, src, b_src):
        for b4 in range(nblocks):
            W, bW = self.next_block(16)
            for pos in range(4):
                oc = b4 * 4 + pos
                ps_, bps = self.bank()
                for kc in range(16):
                    self.mmul(ps_[:], W[:, kc, pos * 128:(pos + 1) * 128], src[:, kc, :], kc == 0, kc == 15, [bW, b_src], [bps], kc == 15)
                self.tt("dve", self.xt[:, oc, :], ps_[:], self.xt[:, oc, :], ALU.add, [bps, self.b_x[oc]], [self.b_x[oc]])

    def mlp(self, rtmp, b_rtmp, mid_hook=None):
        for b16 in range(16):
            W, bW = self.next_block(16)
            for pos in range(4):
                hc = b16 * 4 + pos
                ps_, bps = self.bank()
                for kc in range(16):
                    self.mmul(ps_[:], W[:, kc, pos * 128:(pos + 1) * 128], self.ht[:, kc, :], kc == 0, kc == 15, [bW, self.b_h], [bps], kc == 15)
                r, br = rtmp[hc % 2], b_rtmp[hc % 2]
                self.act(r, ps_[:], AF.Relu, [bps], [br])
                self.tt("pool", self.a[:, hc, :], r, r, ALU.mult, [br], [self.b_a[hc // 16]])
        if mid_hook is not None:
            mid_hook()
        for oc in range(16):
            W, bW = self.next_block(64)
            ps_, bps = self.bank()
            for kc in range(64):
                self.mmul(ps_[:], W[:, kc, :], self.a[:, kc, :], kc == 0, kc == 63, [bW, self.b_a[kc // 16]], [bps], kc == 63)
            self.tt("dve", self.xt[:, oc, :], ps_[:], self.xt[:, oc, :], ALU.add, [bps, self.b_x[oc]], [self.b_x[oc]])


def _dram(nc, name, shape, dty, kind):
    return nc.dram_tensor(name, shape, dty, kind=kind).ap()


NBLK_A = 58
A_TILE_BLOCKS = list(range(58))
NBLK_C = 36
C_TILE_BLOCKS = list(range(36))


def phase_A(nc, p, T):
    xin, xh, wA, ppd, sgd, bsd, wsd = T["xT"], T["xh"], T["wA"], T["ppA"], T["sg"], T["bs"], T["wsT"]
    x1, qloc, kloc, vloc, kmloc, wbf = T["x1"], T["qloc"], T["kloc"], T["vloc"], T["kmloc"], T["wbfA"]
    qT = qloc.rearrange("(h d) t -> d h t", d=128)
    kT = kloc.rearrange("(h d) t -> d h t", d=128)
    vv4 = vloc.rearrange("(h p) (k d) -> p k h d", p=128, d=128)
    kmo = kmloc.rearrange("(h d) k -> d h k", d=128)
    es = ExitStack()
    with es:
        B = TokBuilder(nc, es, p, NBLK_A, wA, wbf, A_TILE_BLOCKS)
        sb = lambda n, s, d: es.enter_context(nc.sbuf_tensor(_uname("sb_" + n), s, d))
        pp = sb("pp", [128, 74], F32)
        sg = sb("sg", [128, 1024], F32)
        bshl = sb("bshl", [33, 1024], BF16)
        ones33 = sb("ones33", [33, 128], BF16)
        wsb = sb("wsb", [128, 8, 128], BF16)
        bsf = B.ar[0:33, 0:1024]
        bst = B.ar[0:33, 1024:2048]
        bshb = B.ar[0:33, 2048:2560].bitcast(BF16)
        wsf = B.ar[:, 3072:4096]
        wsm = B.ar[:, 4096:5120]
        xht = sb("xht", [128, 16, 2], F32)
        hht = sb("hht", [128, 16, 2], BF16)
        gch = sb("gch", [128, 8, 2], F32)
        carry = sb("carry", [128, 8, 2], F32)
        kmacc = sb("kmacc", [128, 16, 16], F32)
        ss4 = [sb("ss4%d" % i, [128, 4], F32) for i in range(2)]
        rt4 = [sb("rt4%d" % i, [128, 4], F32) for i in range(2)]
        rs4 = [sb("rs4%d" % i, [128, 4], F32) for i in range(2)]
        b_ss4 = [Buf("ss4"), Buf("ss4b")]
        b_rt4 = [Buf("rt4"), Buf("rt4b")]
        b_rs4 = [Buf("rs4"), Buf("rs4b")]
        b_pp, b_sg, b_bs, b_ws, b_xh, b_hh = Buf("pp"), Buf("sg"), Buf("bs"), Buf("ws"), Buf("xh"), Buf("hh")
        b_gch = [Buf("gch%d" % i) for i in range(8)]
        b_carry = [Buf("carry%d" % i) for i in range(8)]
        b_km = Buf("km")
        ar = B.ar
        TS = lambda i: ar[:, i * 512:(i + 1) * 512]
        gct = [TS(0), TS(1)]
        cvt = [TS(2), TS(3)]
        ugt = [TS(4), TS(5)]
        vtmp = [TS(6), TS(7)]
        sqv = TS(8)
        yt = [ar[:, 4608:4608 + 514], ar[:, 5632:5632 + 514]]
        b_T = [Buf("T%d" % i) for i in range(12)]
        b_gct, b_cvt, b_ugt, b_vtmp, b_sqv = b_T[0:2], b_T[2:4], b_T[4:6], b_T[6:8], b_T[8]
        b_yt = [[b_T[9], b_T[10]], [b_T[11]]]
        raw = [TS(0), TS(1)]
        b_raw = b_T[0:2]
        kn32 = [TS(2), TS(3)]
        b_kn32 = b_T[2:4]
        qkst = [ar[:, 2048:3072].bitcast(BF16).rearrange("p (h t) -> p h t", h=4),
                ar[:, 3072:4096].bitcast(BF16).rearrange("p (h t) -> p h t", h=4)]
        b_qkst = [[b_T[4], b_T[5]], [b_T[6], b_T[7]]]
        vst = [ar[:, 4096:5120].bitcast(BF16).rearrange("p (h t) -> p h t", h=4),
               ar[:, 5120:6144].bitcast(BF16).rearrange("p (h t) -> p h t", h=4)]
        b_vst = [[b_T[8], b_T[9]], [b_T[10], b_T[11]]]
        vn = B.a[:, 16:24, :].rearrange("p a b -> p (a b)").rearrange("p (t c) -> p t c", t=4)
        b_vn = B.b_a[1]

        p.dma("sp", "c0", lambda e: e.dma_start(out=pp[:], in_=ppd), writes=[b_pp])
        p.dma("sp", "c1", lambda e: e.dma_start(out=sg[:], in_=sgd), writes=[b_sg])
        p.dma("sp", "c2", lambda e: e.dma_start(out=bsf, in_=bsd), writes=[b_bs])
        p.dma("sp", "c3", lambda e: e.dma_start(out=wsf, in_=wsd), writes=[b_ws])
        p.dma("sp", "c4", lambda e: e.dma_start(out=xht[:], in_=xh), writes=[b_xh])
        p.op("pool", lambda e: e.memset(kmacc[:], 0.0), writes=[b_km])
        p.op("dve", lambda e: e.tensor_copy(out=bshb, in_=bsf), reads=[b_bs], writes=[b_bs])
        p.op("dve", lambda e: e.tensor_copy(out=bst, in_=bshb), reads=[b_bs], writes=[b_bs])
        p.op("dve", lambda e: e.tensor_tensor(out=bst, in0=bsf, in1=bst, op=ALU.subtract), reads=[b_bs], writes=[b_bs])
        p.op("dve", lambda e: e.tensor_copy(out=bshl[:], in_=bshb), reads=[b_bs], writes=[b_bs])
        p.op("dve", lambda e: e.tensor_copy(out=bshl[32:33, :], in_=bst[32:33, :]), reads=[b_bs], writes=[b_bs])
        p.op("pool", lambda e: e.memset(ones33[:], 1.0), writes=[b_bs])
        p.op("pool", lambda e: e.affine_select(out=wsm, in_=wsf, pattern=[[0, 8], [1, 128]], compare_op=ALU.is_ge,
                                                fill=0.0, base=0, channel_multiplier=-1), reads=[b_ws], writes=[b_ws])
        p.op("pool", lambda e: e.tensor_copy(out=wsb[:].rearrange("p g i -> p (g i)"), in_=wsm), reads=[b_ws], writes=[b_ws])
        for bt in b_T:
            Prog.merge(bt, [b_bs, b_ws])

        cw = lambda c, tap: pp[:, 48 + c * 3 + tap:48 + c * 3 + tap + 1]
        qg = pp[:, 72:73]
        kg = pp[:, 73:74]

        B.norm(lambda kc: xht[:, kc, :], [b_xh] * 16, 2, pp[:, 0:16], b_pp, lambda kc: hht[:, kc, :], b_hh)

        b_io = Buf("io")
        outs = [T["b_x1"], T["b_qloc"], T["b_kloc"], T["b_vloc"], T["b_kmloc"]]
        for t in range(NTILE):
            t0 = t * TT
            p.dma("sp", "xl", lambda e, t0=t0: e.dma_start(out=B.xt[:], in_=xin[:, :, t0:t0 + TT]), writes=B.b_x)
            B.norm(lambda kc: B.xt[:, kc, :], B.b_x, TT, pp[:, 0:16], b_pp, lambda kc: B.ht[:, kc, :], B.b_h)
            W = bW = None
            for i in range(24):
                blk, pos = divmod(i, 4)
                c, kind = divmod(i, 3)
                if pos == 0:
                    W, bW = B.next_block(16)
                ps_, bps = B.bank()
                for kc in range(16):
                    B.mmul(ps_[:], W[:, kc, pos * 128:(pos + 1) * 128], B.ht[:, kc, :], kc == 0, kc == 15, [bW, B.b_h], [bps], kc == 15)
                if t == 0 and kind < 2:
                    hp, bhp = B.aux[2], B.b_aux[2]
                    for kc in range(16):
                        B.mmul(hp[:, 0:2], W[:, kc, pos * 128:(pos + 1) * 128], hht[:, kc, :], kc == 0, kc == 15, [bW, b_hh], [bhp], kc == 15)
                    if kind == 0:
                        B.copy("act", gch[:, c, :], hp[:, 0:2], [bhp], [b_gch[c]])
                    else:
                        B.tt("dve", carry[:, c, :], hp[:, 0:2], gch[:, c, :], ALU.mult, [bhp, b_gch[c]], [b_carry[c]])
                if kind == 0:
                    B.copy("act", gct[c % 2], ps_[:], [bps], [b_gct[c % 2]])
                elif kind == 1:
                    y, by = yt[c % 2], b_yt[c % 2]
                    B.tt("dve", y[:, 2:514], ps_[:], gct[c % 2], ALU.mult, [bps, b_gct[c % 2]], by)
                    B.copy("pool", y[:, 0:2], carry[:, c, :], [b_carry[c]], by)
                    B.copy("pool", carry[:, c, :], y[:, 512:514], by, [b_carry[c]])
                    cv, bcv = cvt[c % 2], b_cvt[c % 2]
                    B.ts("dve", cv, y[:, 0:512], cw(c, 0), None, ALU.mult, None, by + [b_pp], [bcv])
                    B.stt(cv, y[:, 1:513], cw(c, 1), cv, ALU.mult, ALU.add, by + [b_pp, bcv], [bcv])
                    B.stt(cv, y[:, 2:514], cw(c, 2), cv, ALU.mult, ALU.add, by + [b_pp, bcv], [bcv])
                else:
                    B.tt("dve", B.cat[:, c, :], ps_[:], cvt[c % 2], ALU.mult, [bps, b_cvt[c % 2]], [B.b_cat])
            for vb in range(2):
                W, bW = B.next_block(16)
                for tc in range(4):
                    i2 = vb * 4 + tc
                    ps_, bps = B.bank()
                    for kc in range(16):
                        B.mmul(ps_[:], B.ht[:, kc, tc * 128:(tc + 1) * 128], W[:, kc, :], kc == 0, kc == 15, [bW, B.b_h], [bps], kc == 15)
                    vt, bvt = vtmp[i2 % 2], b_vtmp[i2 % 2]
                    B.act(vt, ps_[:], AF.Gelu_apprx_tanh, [bps], [bvt])
                    B.tt("pool", sqv, vt, vt, ALU.mult, [bvt], [b_sqv])
                    s4, r4, q4 = ss4[i2 % 2], rt4[i2 % 2], rs4[i2 % 2]
                    p.op("dve", lambda e, s4=s4: e.tensor_reduce(out=s4[:], in_=sqv.rearrange("p (g d) -> p g d", g=4), axis=AX.X, op=ALU.add),
                         reads=[b_sqv], writes=[b_ss4[i2 % 2]])
                    B.act(r4[:], s4[:], AF.Sqrt, [b_ss4[i2 % 2], B.b_const], [b_rt4[i2 % 2]], bias=B.epst[:, 0:1], scale=1.0 / 128)
                    p.op("dve", lambda e, r4=r4, q4=q4: e.reciprocal(out=q4[:], in_=r4[:]), reads=[b_rt4[i2 % 2]], writes=[b_rs4[i2 % 2]])
                    for g4 in range(4):
                        c0 = vb * 512 + g4 * 128
                        B.stt(vn[:, tc, c0:c0 + 128], vt[:, g4 * 128:(g4 + 1) * 128], q4[:, g4:g4 + 1], sg[:, c0:c0 + 128], ALU.mult, ALU.mult,
                              [bvt, b_rs4[i2 % 2], b_sg], [b_vn])
            for ub in range(2):
                W, bW = B.next_block(16)
                for pos in range(4):
                    c = ub * 4 + pos
                    ps_, bps = B.bank()
                    for kc in range(16):
                        B.mmul(ps_[:], W[:, kc, pos * 128:(pos + 1) * 128], B.ht[:, kc, :], kc == 0, kc == 15, [bW, B.b_h], [bps], kc == 15)
                    pm, bpm = B.aux[c % 2], B.b_aux[c % 2]
                    for tc in range(4):
                        B.mmul(pm[:, tc * 128:(tc + 1) * 128], vn[:, tc, c * 128:(c + 1) * 128], wsb[:, c, :], True, False, [b_vn, b_ws], [bpm], False)
                        B.mmul(pm[:, tc * 128:(tc + 1) * 128], ones33[:], bshl[:, c * 128:(c + 1) * 128], False, True, [b_bs], [bpm], tc == 3)
                    B.act(ugt[c % 2], ps_[:], AF.Gelu_apprx_tanh, [bps], [b_ugt[c % 2]])
                    B.tt("dve", B.cat[:, 8 + c, :], pm[:], ugt[c % 2], ALU.mult, [bpm, b_ugt[c % 2]], [B.b_cat])
            B.proj_resid(4, B.cat, B.b_cat)
            B.norm(lambda kc: B.xt[:, kc, :], B.b_x, TT, pp[:, 16:32], b_pp, lambda kc: B.ht[:, kc, :], B.b_h)
            B.mlp(gct, b_gct, mid_hook=(lambda t=t: T["convC"](t)) if T.get("convC") is not None else None)
            p.dma("sp", "so_x", lambda e, t0=t0: e.dma_start(out=x1[:, :, t0:t0 + TT], in_=B.xt[:]), reads=B.b_x, writes=[outs[0]])
            B.norm(lambda kc: B.xt[:, kc, :], B.b_x, TT, pp[:, 32:48], b_pp, lambda kc: B.ht[:, kc, :], B.b_h)
            qk_pend = []
            for qb in range(8):
                W, bW = B.next_block(16)
                isk = qb >= 4
                st, bst_ = qkst[qb % 2], b_qkst[qb % 2]
                for pos in range(4):
                    head = (qb % 4) * 4 + pos
                    ps_, bps = B.bank()
                    for kc in range(16):
                        B.mmul(ps_[:], W[:, kc, pos * 128:(pos + 1) * 128], B.ht[:, kc, :], kc == 0, kc == 15, [bW, B.b_h], [bps], kc == 15)
                    rw, brw = raw[pos % 2], b_raw[pos % 2]
                    B.copy("act", rw, ps_[:], [bps], [brw])
                    s, sbf = B.sq[pos % 2], B.b_sq[pos % 2]
                    B.tt("pool", s[:], rw, rw, ALU.mult, [brw], [sbf])
                    def tail(pos=pos, s=s, sbf=sbf, rw=rw, brw=brw, st=st, bst_=bst_, isk=isk, head=head, t=t):
                        pn, bpn = B.aux[pos % 2], B.b_aux[pos % 2]
                        B.mmul(pn[:], B.ones_bf[:], s[:], True, True, [sbf, B.b_const], [bpn], True)
                        B.rms_rstd(pn[:], bpn, TT, 1.0 / 128, B.rt, B.b_rt, B.rstd, B.b_rstd)
                        kn, bkn = kn32[pos % 2], b_kn32[pos % 2]
                        B.stt(kn, rw, kg if isk else qg, B.rstd[:], ALU.mult, ALU.mult, [brw, B.b_rstd, b_pp], [bkn])
                        B.copy("pool", st[:, pos, :], kn, [bkn], bst_)
                        if isk:
                            p.op("dve", lambda e, kn=kn, head=head, t=t: e.tensor_reduce(
                                out=kmacc[:, head, 2 * t:2 * t + 2], in_=kn.rearrange("p (b k) -> p b k", b=2), axis=AX.X, op=ALU.add),
                                reads=[bkn], writes=[b_km])
                    qk_pend.append(tail)
                    if len(qk_pend) > 1:
                        qk_pend.pop(0)()
                while qk_pend:
                    qk_pend.pop(0)()
                dst = kloc if isk else qloc
                h0 = (qb % 4) * 4
                row0 = (t * 2 + h0 // 8) * 1024 + (h0 % 8) * 128
                p.dma("sp", "so_q%d" % (qb % 2), lambda e, dst=dst, row0=row0, st=st: e.dma_start(
                    out=dst[row0:row0 + 512, :].rearrange("(hh d) c -> d hh c", d=128), in_=st),
                      reads=bst_, writes=[outs[2 if isk else 1]])
            for vb in range(4):
                W, bW = B.next_block(16)
                st, bst_ = vst[vb % 2], b_vst[vb % 2]
                for tc in range(4):
                    ps_, bps = B.bank()
                    for kc in range(16):
                        B.mmul(ps_[:], B.ht[:, kc, tc * 128:(tc + 1) * 128], W[:, kc, :], kc == 0, kc == 15, [bW, B.b_h], [bps], kc == 15)
                    B.copy("act", st[:, tc, :], ps_[:], [bps], bst_)
                for hh in range(4):
                    p.dma("sp", "so_v%d" % (vb % 2), lambda e, st=st, vb=vb, t=t, hh=hh: e.dma_start(
                        out=vloc[(t * 2 + (4 * vb + hh) // 8) * 1024 + ((4 * vb + hh) % 8) * 128:(t * 2 + (4 * vb + hh) // 8) * 1024 + ((4 * vb + hh) % 8) * 128 + 128, :].rearrange("p (k d) -> p k d", d=128),
                        in_=st[:, :, hh * 128:(hh + 1) * 128]),
                        reads=bst_, writes=[outs[3]])
            T["gatherA"](t)

        p.op("act", lambda e: e.mul(out=kmacc[:], in_=kmacc[:], mul=1.0 / 256), reads=[b_km], writes=[b_km])
        p.dma("sp", "so_km", lambda e: e.dma_start(out=kmo, in_=kmacc[:]), reads=[b_km], writes=[outs[4]])
        T["gatherKM"]()
        p.barrier()
        p.emit()


def phase_C(nc, p, T):
    x1, og, wbf, ppd, xo, idxd = T["x1"], T["og"], T["wbfC"], T["ppC"], T["xo"], T["idxC"]
    es = ExitStack()
    with es:
        B = TokBuilder(nc, es, p, NBLK_C, None, wbf, C_TILE_BLOCKS, b_wbf=[T["b_wbfC"]] * NBLK_C)
        pp = es.enter_context(nc.sbuf_tensor("sb_ppc", [128, 16], F32))
        idx = es.enter_context(nc.sbuf_tensor("sb_idxc", [128, 128], U32))
        b_pp = Buf("pp")
        p.dma("sp", "c0", lambda e: e.dma_start(out=pp[:], in_=ppd), writes=[b_pp])
        p.dma("sp", "c1", lambda e: e.dma_start(out=idx[:], in_=idxd), writes=[b_pp])
        rtmp = [B.ar[:, 0:512], B.ar[:, 512:1024]]
        b_rtmp = [Buf("r0"), Buf("r1")]
        b_out = Buf("out")
        for t in range(NTILE):
            t0 = t * TT
            p.dma("sp", "xl", lambda e, t0=t0: e.dma_start(out=B.xt[:], in_=x1[:, :, t0:t0 + TT]), reads=[T["b_x1"]], writes=B.b_x)
            for h in range(16):
                p.dma("pool", "al%d" % (h % 2), lambda e, h=h, t=t: e.indirect_dma_start(
                    out=B.cat[:, h, :], out_offset=None, in_=og,
                    in_offset=bass.IndirectOffsetOnAxis(ap=idx[:, t * 16 + h:t * 16 + h + 1], axis=0)),
                    reads=[T["b_og"], b_pp], writes=[B.b_cat])
            B.proj_resid(4, B.cat, B.b_cat)
            B.norm(lambda kc: B.xt[:, kc, :], B.b_x, TT, pp[:, 0:16], b_pp, lambda kc: B.ht[:, kc, :], B.b_h)
            B.mlp(rtmp, b_rtmp)
            p.dma("sp", "so_x", lambda e, t0=t0: e.dma_start(out=xo[:, :, t0:t0 + TT], in_=B.xt[:]), reads=B.b_x, writes=[b_out])
        p.wait_all_dma("sp")
        p.barrier()
        p.emit()


NPAIR = 4
SCALE = 128 ** -0.5
BIGS = 30000.0 / SCALE
ALIBI_THR = 196.0
ALIBI_WIN = [ALIBI_THR * 2.0 ** (2 * i + 2) for i in range(4)]


def phase_B(nc, p, T, npair=NPAIR, nqt=S // TT):
    qg, kg, vg, kmg, sld, oloc, idxd = T["qg"], T["kg"], T["vg"], T["kmg"], T["slope"], T["oloc"], T["idxB"]
    es = ExitStack()
    with es:
        sb = lambda n, s, d: es.enter_context(nc.sbuf_tensor(_uname("sb_" + n), s, d))
        psm = lambda n, d=F32: es.enter_context(nc.psum_tensor(_uname(n), [128, 512], d))
        qT = sb("qT", [128, S], BF16)
        kT = sb("kT", [128, S], BF16)
        vt = sb("vt", [128, 128, 128], BF16)
        kmf = sb("kmf", [128, 64], F32)
        kmb = sb("kmb", [128, 64], BF16)
        slope = sb("slope", [128, npair], F32)
        idx = sb("idxb", [128, 128], U32)
        idxk = sb("idxk", [128, 16], U32)
        ident = sb("ident", [128, 128], BF16)
        ones32 = sb("ones32", [128, 128], F32)
        E = sb("E", [65, 65, 128], BF16)
        iot = sb("iot", [128, 131], F32)
        kb = sb("kb", [128, npair, 131], F32)
        iot4 = sb("iot4", [128, 4], F32)
        qterm = sb("qterm", [128, npair, 4], F32)
        gm = [sb("gm%d" % i, [128, 64], F32) for i in range(2)]
        mx = [sb("mx%d" % i, [128, 8], F32) for i in range(2)]
        nm32 = [sb("nm32%d" % i, [128, 65], F32) for i in range(2)]
        nmb = [sb("nmb%d" % i, [128, 65], BF16) for i in range(8)]
        NMT = [sb("NMT%d" % i, [65, 512], BF16) for i in range(2)]
        pT = [sb("pT%d" % i, [128, 512], BF16) for i in range(3)]
        acc = sb("acc", [128, 512], F32)
        rl = sb("rl", [128, 512], F32)
        ot = [sb("ot%d" % i, [128, 512], BF16) for i in range(2)]
        ps_s = [psm("ps_s%d" % i) for i in range(3)]
        ps_o = [psm("ps_o%d" % i) for i in range(2)]
        ps_l = psm("ps_l")
        pg = psm("pg")
        pt = psm("pt", BF16)
        b_q, b_k, b_v, b_km, b_c = Buf("q"), Buf("k"), Buf("v"), Buf("km"), Buf("c")
        b_gm = [Buf("gm0"), Buf("gm1")]
        b_mx = [Buf("mx0"), Buf("mx1")]
        b_nm32 = [Buf("nm320"), Buf("nm321")]
        b_nmb = [Buf("nmb%d" % i) for i in range(8)]
        b_NMT = [Buf("NMT0"), Buf("NMT1")]
        b_pT = [Buf("pT%d" % i) for i in range(3)]
        b_acc, b_rl = Buf("acc"), Buf("rl")
        b_ot = [Buf("ot0"), Buf("ot1")]
        b_ps_s = [Buf("pss%d" % i) for i in range(3)]
        b_ps_o = [Buf("pso%d" % i) for i in range(2)]
        b_ps_l, b_pg, b_pt = Buf("psl"), Buf("pg"), Buf("pt")
        b_out = Buf("out")

        p.dma("sp", "c0", lambda e: e.dma_start(out=slope[:], in_=sld), writes=[b_c])
        p.dma("sp", "c1", lambda e: e.dma_start(out=idx[:], in_=idxd), writes=[b_c])
        p.dma("sp", "c2", lambda e: e.dma_start(out=idxk[:], in_=T["idxK"]), writes=[b_c])
        p.op("pool", lambda e: e.memset(ident[:], 1.0), writes=[b_c])
        p.op("pool", lambda e: e.affine_select(out=ident[:], in_=ident[:], pattern=[[1, 128]], compare_op=ALU.is_equal, fill=0.0,
                                                base=0, channel_multiplier=-1), reads=[b_c], writes=[b_c])
        p.op("pool", lambda e: e.memset(ones32[:], 1.0), writes=[b_c])
        p.op("pool", lambda e: e.memset(E[:], 1.0), writes=[b_c])
        p.op("pool", lambda e: e.affine_select(out=E[:], in_=E[:], pattern=[[1, 65], [0, 128]], compare_op=ALU.is_equal, fill=0.0,
                                                base=0, channel_multiplier=-1), reads=[b_c], writes=[b_c])
        p.op("pool", lambda e: e.iota(iot[:], pattern=[[128, 131]], base=-127 * 128, channel_multiplier=1,
                                       allow_small_or_imprecise_dtypes=True), writes=[b_c])
        p.op("pool", lambda e: e.iota(iot4[:], pattern=[[128, 4]], base=0, channel_multiplier=1,
                                       allow_small_or_imprecise_dtypes=True), writes=[b_c])
        for i in range(npair):
            p.op("dve", lambda e, i=i: e.tensor_scalar(out=kb[:, i, :], in0=iot[:], scalar1=slope[:, i:i + 1], scalar2=None, op0=ALU.mult),
                 reads=[b_c], writes=[b_c])
            p.op("dve", lambda e, i=i: e.tensor_scalar(out=qterm[:, i, :], in0=iot4[:], scalar1=slope[:, i:i + 1], scalar2=-1.0 / SCALE,
                                                        op0=ALU.mult, op1=ALU.mult), reads=[b_c], writes=[b_c])
        for i in range(2):
            p.op("pool", lambda e, i=i: e.memset(nm32[i][:], 0.0), writes=[b_nm32[i]])

        def gateA(pi, qt):
            par = qt % 2
            for j in range(4):
                own = 2 * qt + j // 2
                g2 = j % 2
                qs = qT[:, qt * TT + j * 128: qt * TT + (j + 1) * 128]
                p.op("pe", lambda e, qs=qs: e.matmul(pg[:, 0:64], lhsT=qs, rhs=kmb[:], start=True, stop=True), reads=[b_q, b_km], writes=[b_pg])
                if own > 0:
                    p.op("act", lambda e, g2=g2, own=own: e.copy(out=gm[g2][:, 0:own], in_=pg[:, 0:own]), reads=[b_pg], writes=[b_gm[g2]])
                p.op("dve", lambda e, g2=g2: e.max(out=mx[g2][:], in_=gm[g2][:]), reads=[b_gm[g2]], writes=[b_mx[g2]])
                p.op("dve", lambda e, g2=g2: e.tensor_scalar(out=nm32[g2][:, 0:64], in0=gm[g2][:], scalar1=mx[g2][:, 2:3], scalar2=-BIGS,
                                                             op0=ALU.is_lt, op1=ALU.mult), reads=[b_gm[g2], b_mx[g2]], writes=[b_nm32[g2]])
                nb_ = nmb[par * 4 + j]
                p.op("dve", lambda e, g2=g2, nb_=nb_, j=j: e.tensor_scalar(out=nb_[:], in0=nm32[g2][:], scalar1=qterm[:, pi, j:j + 1], scalar2=None,
                                                                          op0=ALU.add), reads=[b_nm32[g2], b_c], writes=[b_nmb[par * 4 + j]])

        def gateB(pi, qt):
            par = qt % 2
            for j in range(4):
                nb_ = nmb[par * 4 + j]
                p.op("pe", lambda e, nb_=nb_, j=j: e.transpose(out=pt[0:65, j * 128:(j + 1) * 128], in_=nb_[:], identity=ident[:]),
                     reads=[b_nmb[par * 4 + j], b_c], writes=[b_pt])
            p.op("act", lambda e: e.copy(out=NMT[par][:], in_=pt[0:65, :]), reads=[b_pt], writes=[b_NMT[par]])

        step = [0]

        def keyloop(pi, qt):
            par = qt % 2
            po, bpo = ps_o[par], b_ps_o[par]
            nkt = 4 * qt + 4
            pend = []
            kts = [kt for kt in range(nkt) if not (kt < 4 * qt and (qt * TT - (kt * 128 + 127)) > ALIBI_WIN[pi])]
            for ki, kt in enumerate(kts):
                kfirst, klast = ki == 0, ki == len(kts) - 1
                n = kt // 2
                iin = kt - 4 * qt
                if iin < 0:
                    ranges = [(0, 512, n)]
                elif iin == 0:
                    ranges = [(0, 256, 64), (256, 512, n)]
                elif iin == 1:
                    ranges = [(128, 256, 64), (256, 512, n)]
                elif iin == 2:
                    ranges = [(256, 512, 64)]
                else:
                    ranges = [(384, 512, 64)]
                c0 = ranges[0][0]
                si = step[0] % 3
                step[0] += 1
                pss, bpss = ps_s[si], b_ps_s[si]
                pTt, bpT = pT[si], b_pT[si]
                q0 = qt * TT
                p.op("pe", lambda e, pss=pss, kt=kt, c0=c0, q0=q0: e.matmul(pss[:, c0:512], lhsT=kT[:, kt * 128:(kt + 1) * 128],
                                                                         rhs=qT[:, q0 + c0:q0 + 512], start=True, stop=False),
                     reads=[b_k, b_q], writes=[bpss], inc=False)
                for ri, (a_, b_, row) in enumerate(ranges):
                    last = ri == len(ranges) - 1
                    p.op("pe", lambda e, pss=pss, a_=a_, b_=b_, row=row, last=last: e.matmul(
                        pss[:, a_:b_], lhsT=E[:, row, :], rhs=NMT[par][:, a_:b_], start=False, stop=last),
                        reads=[b_c, b_NMT[par]], writes=[bpss], inc=last)
                while pend:
                    pend.pop(0)()
                mi = kt - 4 * qt + 127
                p.op("act", lambda e, pss=pss, pTt=pTt, c0=c0, mi=mi: e.activation(out=pTt[:, c0:512], in_=pss[:, c0:512], func=AF.Exp,
                                                                                 bias=kb[:, pi, mi:mi + 1], scale=SCALE),
                     reads=[bpss, b_c], writes=[bpT])
                if iin >= 0:
                    j = iin
                    p.op("pool", lambda e, pTt=pTt, j=j: e.affine_select(out=pTt[:, j * 128:(j + 1) * 128], in_=pTt[:, j * 128:(j + 1) * 128],
                                                                       pattern=[[1, 128]], compare_op=ALU.is_ge, fill=0.0, base=0,
                                                                       channel_multiplier=-1), reads=[bpT], writes=[bpT])
                if kfirst:
                    assert c0 == 0
                    p.op("dve", lambda e, pTt=pTt: e.tensor_copy(out=acc[:], in_=pTt[:]), reads=[bpT], writes=[b_acc])
                else:
                    p.op("dve", lambda e, pTt=pTt, c0=c0: e.tensor_tensor(out=acc[:, c0:512], in0=acc[:, c0:512], in1=pTt[:, c0:512], op=ALU.add),
                         reads=[bpT, b_acc], writes=[b_acc])
                pend.append(lambda po=po, kt=kt, c0=c0, pTt=pTt, kfirst=kfirst, klast=klast, bpT=bpT: p.op(
                    "pe", lambda e: e.matmul(po[:, c0:512], lhsT=vt[:, kt, :], rhs=pTt[:, c0:512], start=kfirst, stop=klast),
                    reads=[b_v, bpT], writes=[bpo], inc=True))
            while pend:
                pend.pop(0)()
            p.op("pe", lambda e: e.matmul(ps_l[:], lhsT=ones32[:], rhs=acc[:], start=True, stop=True), reads=[b_acc, b_c], writes=[b_ps_l])
            p.op("dve", lambda e: e.reciprocal(out=rl[:], in_=ps_l[:]), reads=[b_ps_l], writes=[b_rl])
            o_, bo = ot[par], b_ot[par]
            p.op("dve", lambda e, o_=o_, po=po: e.tensor_tensor(out=o_[:], in0=po[:], in1=rl[:], op=ALU.mult), reads=[bpo, b_rl], writes=[bo])
            row0 = (pi * 32 + qt) * 128
            p.dma("sp", "so%d" % par, lambda e, o_=o_, row0=row0: e.dma_start(out=oloc[row0:row0 + 128, :], in_=o_[:]), reads=[bo], writes=[T["b_oloc"]])
            if qt % 8 == 7:
                T["gatherB"](pi * 4 + qt // 8)

        for pi in range(npair):
            vflat = vt[:].rearrange("p t d -> p (t d)")
            first = True
            for r in range(4):
                iok = bass.IndirectOffsetOnAxis(ap=idxk[:, pi * 4 + r:pi * 4 + r + 1], axis=0)
                p.dma("pool", "lm", lambda e, r=r, iok=iok: e.indirect_dma_start(out=kmf[:, r * 16:(r + 1) * 16], out_offset=None, in_=kmg, in_offset=iok),
                      reads=[T["b_kmg"], b_c] + ([T["b_og"]] if (first and pi > 0) else []), writes=[b_km] if first else [])
                for t in range(NTILE):
                    col = (pi * 4 + r) * 8 + t
                    io = bass.IndirectOffsetOnAxis(ap=idx[:, col:col + 1], axis=0)
                    c0 = r * TOK + t * TT
                    p.dma("pool", "lq", lambda e, c0=c0, io=io: e.indirect_dma_start(out=qT[:, c0:c0 + TT], out_offset=None, in_=qg, in_offset=io),
                          reads=[T["b_qg"], b_c], writes=[b_q] if first else [])
                    p.dma("pool", "lk", lambda e, c0=c0, io=io: e.indirect_dma_start(out=kT[:, c0:c0 + TT], out_offset=None, in_=kg, in_offset=io),
                          reads=[T["b_kg"], b_c], writes=[b_k] if first else [])
                    p.dma("pool", "lv", lambda e, c0=c0, io=io: e.indirect_dma_start(out=vflat[:, c0:c0 + TT], out_offset=None, in_=vg, in_offset=io),
                          reads=[T["b_vg"], b_c], writes=[b_v] if first else [])
                    first = False
            b_q.w, b_k.w, b_v.w, b_km.w = ("lq", p.cnt["lq"]), ("lk", p.cnt["lk"]), ("lv", p.cnt["lv"]), ("lm", p.cnt["lm"])
            p.op("dve", lambda e: e.tensor_copy(out=kmb[:], in_=kmf[:]), reads=[b_km], writes=[b_km])
            for i in range(2):
                p.op("pool", lambda e, i=i: e.memset(gm[i][:], NEG), writes=[b_gm[i]])
            gateA(pi, 0)
            gateB(pi, 0)
            for qt in range(nqt):
                if qt + 1 < nqt:
                    gateA(pi, qt + 1)
                keyloop(pi, qt)
                if qt + 1 < nqt:
                    gateB(pi, qt + 1)
        p.barrier()
        p.emit()


def _tile16(W, n0, ncols=512):
    return np.ascontiguousarray(W[:, n0:n0 + ncols].reshape(16, 128, ncols).transpose(1, 0, 2)).reshape(128, 16 * ncols)


def _tile64(W, n0):
    return np.ascontiguousarray(W[:, n0:n0 + 128].reshape(64, 128, 128).transpose(1, 0, 2)).reshape(128, 8192)


def _fm(v):
    return np.ascontiguousarray(v.reshape(16, 128).T)


def _slopes():
    return (2.0 ** (-8.0 * np.arange(1, 17, dtype=np.float32) / 16)).astype(np.float32)


_CACHE = {}


def _get(name, fn):
    if name not in _CACHE:
        _CACHE[name] = fn()
    return _CACHE[name]


def prep_A(inp):
    x = inp["x"]
    w_in = inp["w_in"][0]
    order = []
    for c in range(8):
        order += list(range(1024 + 128 * c, 1024 + 128 * (c + 1)))
        order += list(range(2048 + 128 * c, 2048 + 128 * (c + 1)))
        order += list(range(0 + 128 * c, 0 + 128 * (c + 1)))
    order += list(range(4096, 5120))
    order += list(range(3072, 4096))
    w_in_p = w_in[:, order]
    blocks = [_tile16(w_in_p, n0) for n0 in range(0, 5120, 512)]
    blocks += [_tile16(inp["w_mix_out"][0], n0) for n0 in range(0, 2048, 512)]
    blocks += [_tile16(inp["w_up"][0], n0) for n0 in range(0, 8192, 512)]
    blocks += [_tile64(inp["w_down"][0], n0) for n0 in range(0, 2048, 128)]
    blocks += [_tile16(inp["w_qkv"][0], n0) for n0 in range(0, 6144, 512)]
    wA = np.stack(blocks).astype(np.float32)
    assert wA.shape == (NBLK_A, 128, 8192)
    pp = np.zeros((128, 74), np.float32)
    pp[:, 0:16] = _fm(inp["mix_norm"][0])
    pp[:, 16:32] = _fm(inp["ffn_norm"][0])
    pp[:, 32:48] = _fm(inp["mix_norm"][1])
    pp[:, 48:72] = inp["conv_w"][0].reshape(8, 128, 3).transpose(1, 0, 2).reshape(128, 24)
    pp[:, 72] = inp["q_gain"][0]
    pp[:, 73] = inp["k_gain"][0]
    sg = np.ascontiguousarray(np.broadcast_to(inp["sgu_gain"][0][None, :], (128, 1024))).astype(np.float32)
    bs = np.zeros((33, 1024), np.float32)
    bs[0] = inp["b_s"][0].reshape(1024)
    bs[32] = inp["b_s"][0].reshape(1024)
    wsT = np.ascontiguousarray(inp["w_s"][0].transpose(2, 0, 1)).reshape(128, 1024).astype(np.float32)
    maps = []
    for c in range(8):
        b, r = divmod(c, 4)
        s0 = r * TOK
        xs = x[b, s0:s0 + TOK]
        xT = np.ascontiguousarray(xs.reshape(TOK, 16, 128).transpose(2, 1, 0))
        xh = np.zeros((128, 16, 2), np.float32)
        if r > 0:
            xh[:] = x[b, s0 - 2:s0].reshape(2, 16, 128).transpose(2, 1, 0)
        maps.append({"xT": xT, "xh": xh, "wA": wA, "pp": pp, "sg": sg, "bs": bs, "wsT": wsT})
    return maps


def build_F():
    nc = bass.Bass("TRN2", target_bir_lowering=False)
    T = {}
    ext = lambda n, sh, dt: _dram(nc, n, sh, dt, "ExternalInput")
    itn = lambda n, sh, dt: _dram(nc, n, sh, dt, "Internal")
    T["xT"] = ext("xT", [128, 16, TOK], F32)
    T["xh"] = ext("xh", [128, 16, 2], F32)
    T["wA"] = ext("wA", [NBLK_A, 128, 8192], F32)
    T["wC"] = ext("wC", [NBLK_C, 128, 8192], F32)
    T["ppA"] = ext("pp", [128, 74], F32)
    T["ppC"] = ext("ppC", [128, 16], F32)
    T["sg"] = ext("sg", [128, 1024], F32)
    T["bs"] = ext("bs", [33, 1024], F32)
    T["wsT"] = ext("wsT", [128, 1024], F32)
    T["slope"] = ext("slope", [128, NPAIR], F32)
    T["idxB"] = ext("idxB", [128, 128], U32)
    T["idxK"] = ext("idxK", [128, 16], U32)
    T["idxC"] = ext("idxC", [128, 128], U32)
    T["xo"] = _dram(nc, "xo", [128, 16, TOK], F32, "ExternalOutput")
    T["wbfA"] = itn("wbfA", [NBLK_A, 128, 8192], BF16)
    T["wbfC"] = itn("wbfC", [NBLK_C, 128, 8192], BF16)
    T["x1"] = itn("x1", [128, 16, TOK], F32)
    for nm in ("q", "k", "v"):
        T[nm + "loc"] = itn(nm + "loc", [16 * 1024, TT], BF16)
        T[nm + "g"] = itn(nm + "g", [16 * 4096, TT], BF16)
    T["kmloc"] = itn("kmloc", [2048, 16], F32)
    T["kmg"] = itn("kmg", [4 * 2048, 16], F32)
    T["oloc"] = itn("oloc", [NPAIR * 32 * 128, TT], BF16)
    T["og"] = itn("og", [4 * NPAIR * 32 * 128, TT], BF16)
    for nm in ("x1", "qloc", "kloc", "vloc", "kmloc", "qg", "kg", "vg", "kmg", "oloc", "og", "wbfC"):
        T["b_" + nm] = Buf(nm)
    groups = [[0, 1, 2, 3], [4, 5, 6, 7]]
    es = ExitStack()
    with es:
        p = Prog(nc, es)

        def convC(t):
            for i in range(t * 5, min((t + 1) * 5, NBLK_C)):
                p.dma("pool", "cvC", lambda e, i=i: e.dma_start(out=T["wbfC"][i], in_=T["wC"][i]),
                      reads=[T["b_qg"], T["b_kg"], T["b_vg"]] if i == t * 5 else [])
            T["b_wbfC"].w = ("cvC", p.cnt["cvC"])

        def gatherA(t):
            for nm in ("q", "k", "v"):
                for hh in range(2):
                    c = t * 2 + hh
                    p.dma("pool", "cc" + nm, lambda e, nm=nm, c=c: e.collective_compute(
                        "AllGather", ALU.bypass, replica_groups=groups,
                        ins=[T[nm + "loc"][c * 1024:(c + 1) * 1024, :].opt()], outs=[T[nm + "g"][c * 4096:(c + 1) * 4096, :].opt()]),
                        reads=[T["b_" + nm + "loc"]], writes=[], amt=1)
                T["b_" + nm + "g"].w = ("cc" + nm, p.cnt["cc" + nm])

        def gatherKM():
            p.dma("pool", "cckm", lambda e: e.collective_compute(
                "AllGather", ALU.bypass, replica_groups=groups, ins=[T["kmloc"].opt()], outs=[T["kmg"].opt()]),
                reads=[T["b_kmloc"]], writes=[T["b_kmg"]], amt=1)

        def gatherB(c):
            p.dma("pool", "cco", lambda e, c=c: e.collective_compute(
                "AllGather", ALU.bypass, replica_groups=groups,
                ins=[T["oloc"][c * 1024:(c + 1) * 1024, :].opt()], outs=[T["og"][c * 4096:(c + 1) * 4096, :].opt()]),
                reads=[T["b_oloc"]], writes=[], amt=1)
            T["b_og"].w = ("cco", p.cnt["cco"])

        T["gatherKM"] = gatherKM
        T["convC"], T["gatherA"], T["gatherB"] = convC, gatherA, gatherB
        phase_A(nc, p, T)
        phase_B(nc, p, T)
        phase_C(nc, p, T)
    return nc


def prep_F(inp):
    maps = prep_A(inp)
    blocks = [_tile16(inp["w_attn_out"][0], n0) for n0 in range(0, 2048, 512)]
    blocks += [_tile16(inp["w_up"][1], n0) for n0 in range(0, 8192, 512)]
    blocks += [_tile64(inp["w_down"][1], n0) for n0 in range(0, 2048, 128)]
    wC = np.stack(blocks).astype(np.float32)
    ppC = _fm(inp["ffn_norm"][1]).astype(np.float32)
    sl = _slopes()
    pidx = np.arange(128, dtype=np.int64)
    for c in range(8):
        b, r = divmod(c, 4)
        hq = r
        slope = np.zeros((128, NPAIR), np.float32)
        idxB = np.zeros((128, 128), np.uint32)
        idxK = np.zeros((128, 16), np.uint32)
        for i in range(NPAIR):
            h = 4 * i + hq
            slope[:, i] = sl[h]
            for rr in range(4):
                idxK[:, i * 4 + rr] = rr * 2048 + h * 128 + pidx
                for t in range(NTILE):
                    idxB[:, (i * 4 + rr) * 8 + t] = ((t * 2 + h // 8) * 4 + rr) * 1024 + (h % 8) * 128 + pidx
        idxC = np.zeros((128, 128), np.uint32)
        for t in range(NTILE):
            for h in range(16):
                idxC[:, t * 16 + h] = ((h // 4) * 4 + r) * 4096 + (h % 4) * 1024 + t * 128 + pidx
        maps[c].update({"wC": wC, "ppC": ppC, "slope": slope, "idxB": idxB, "idxK": idxK, "idxC": idxC})
    return maps


def kernel(**inputs):
    inp = {k: np.asarray(v) for k, v in inputs.items()}
    nc = _get("F", build_F)
    res = run_bass_kernel_spmd(nc, prep_F(inp), core_ids=list(range(8))).results
    out = np.empty((NB, S, D), np.float32)
    for c in range(8):
        b, r = divmod(c, 4)
        out[b, r * TOK:(r + 1) * TOK] = np.asarray(res[c]["xo"]).transpose(2, 1, 0).reshape(TOK, D)
    return out
```
